# Optimizing a Trainium2 kernel written in Bass

```python
import jax, jax.numpy as jnp
from jax import lax
import numpy as np


D_MODEL = 1024
BATCH = 4
SEQ = 4096
DEPTH = 4

GLA_HEADS = 4
GLA_DK = 64
GLA_DV = 128
GLA_RANK = 16
GLA_TAU = 16.0
GLA_CHUNK = 64
ML_HEADS = 4
ML_DK = 64
ML_DV = 128
ML_CONV = 4
ML_CHUNK = 64
MOBA_HEADS = 8
MOBA_HD = D_MODEL // MOBA_HEADS
MOBA_BLOCK = 256
MOBA_TOPK = 3
MOBA_QCHUNK = 16
D_FF = -(-8 * D_MODEL // (3 * 256)) * 256

N_EVEN = (DEPTH + 1) // 2
N_ODD = DEPTH // 2
GLA_QK = GLA_HEADS * GLA_DK
GLA_V = GLA_HEADS * GLA_DV
ML_QK = ML_HEADS * ML_DK
ML_V = ML_HEADS * ML_DV
HYB_SPLITS = (GLA_QK, GLA_QK, GLA_V, GLA_V, GLA_RANK, ML_QK, ML_QK, ML_V, ML_V, ML_HEADS, ML_HEADS)
HYB_IN = sum(HYB_SPLITS)
EPS = 1e-6
NEG = -1e30

kernel_name = 'hybrid_gla_mlstm_moba_swiglu'


def rms_norm(x, g):
    xf = x.astype(jnp.float32)
    y = xf * lax.rsqrt(jnp.mean(xf * xf, axis=-1, keepdims=True) + EPS)
    return (y * g.astype(jnp.float32)).astype(x.dtype)


def split_heads(t, h):
    b, s, _ = t.shape
    return t.reshape(b, s, h, -1).transpose(0, 2, 1, 3)


def merge_heads(t):
    b, h, s, d = t.shape
    return t.transpose(0, 2, 1, 3).reshape(b, s, h * d)


def to_chunks(t, c):
    b, h, s = t.shape[:3]
    return jnp.moveaxis(t.reshape(b, h, s // c, c, *t.shape[3:]), 2, 0)


def from_chunks(t):
    n, b, h, c = t.shape[:4]
    return jnp.moveaxis(t, 0, 2).reshape(b, h, n * c, *t.shape[4:])


def causal_conv(x, w):
    c = x.shape[-1]
    return lax.conv_general_dilated(x, w[:, None, :].astype(x.dtype), (1,), [(w.shape[0] - 1, 0)],
                                    dimension_numbers=('NWC', 'WIO', 'NWC'), feature_group_count=c)


def alibi_slopes(n):
    return jnp.asarray([2.0 ** (-8.0 * (i + 1) / n) for i in range(n)], jnp.float32)


def gla_chunked(q, k, v, log_a):
    b, h, s, dk = q.shape
    dv = v.shape[-1]
    c = GLA_CHUNK
    mask = jnp.tril(jnp.ones((c, c), bool))[:, :, None]

    def step(state, inp):
        qc, kc, vc, gc = inp
        bc = jnp.cumsum(gc, axis=2)
        diff = bc[:, :, :, None, :] - bc[:, :, None, :, :]
        decay = jnp.where(mask, jnp.exp(jnp.where(mask, diff, 0.0)), 0.0)
        scores = jnp.einsum('bhtk,bhsk,bhtsk->bhts', qc, kc, decay)
        out = (jnp.einsum('bhts,bhsv->bhtv', scores, vc)
               + jnp.einsum('bhtk,bhkv->bhtv', qc * jnp.exp(bc), state))
        last = bc[:, :, -1:, :]
        state = (jnp.exp(last[:, :, 0, :])[..., None] * state
                 + jnp.einsum('bhsk,bhsv->bhkv', kc * jnp.exp(last - bc), vc))
        return state, out

    state0 = jnp.zeros((b, h, dk, dv), jnp.float32)
    _, out = lax.scan(step, state0, tuple(to_chunks(t, c) for t in (q, k, v, log_a)))
    return from_chunks(out)


def mlstm_chunked(q, k, v, i_pre, log_f):
    b, h, s, dk = q.shape
    dv = v.shape[-1]
    c = ML_CHUNK
    mask = jnp.tril(jnp.ones((c, c), bool))

    def step(carry, inp):
        cs, ns, m = carry
        qc, kc, vc, ic, fc = inp
        bc = jnp.cumsum(fc, axis=-1)
        dmat = jnp.where(mask, bc[..., :, None] - bc[..., None, :] + ic[..., None, :], -jnp.inf)
        inter_log = bc + m[..., None]
        m_t = jnp.maximum(inter_log, jnp.max(dmat, axis=-1))
        w = jnp.einsum('bhtk,bhsk->bhts', qc, kc) * jnp.exp(dmat - m_t[..., None])
        inter = jnp.exp(inter_log - m_t)
        num = inter[..., None] * jnp.einsum('bhtk,bhkv->bhtv', qc, cs) + jnp.einsum('bhts,bhsv->bhtv', w, vc)
        den = inter * jnp.einsum('bhtk,bhk->bht', qc, ns) + jnp.sum(w, axis=-1)
        hout = num / jnp.maximum(jnp.abs(den), jnp.exp(-m_t))[..., None]
        g = bc[..., -1:] - bc + ic
        m_new = jnp.maximum(bc[..., -1] + m, jnp.max(g, axis=-1))
        carry_scale = jnp.exp(bc[..., -1] + m - m_new)
        wk = kc * jnp.exp(g - m_new[..., None])[..., None]
        cs = carry_scale[..., None, None] * cs + jnp.einsum('bhsk,bhsv->bhkv', wk, vc)
        ns = carry_scale[..., None] * ns + jnp.sum(wk, axis=2)
        return (cs, ns, m_new), hout

    carry0 = (jnp.zeros((b, h, dk, dv), jnp.float32), jnp.zeros((b, h, dk), jnp.float32),
              jnp.zeros((b, h), jnp.float32))
    _, out = lax.scan(step, carry0, tuple(to_chunks(t, c) for t in (q, k, v, i_pre, log_f)))
    return from_chunks(out)


def hybrid_mixer(xn, w_in, w_gate2, b_gate, gla_gain, conv_w, b_if, ml_gain, w_out):
    b, s, _ = xn.shape
    f32 = jnp.float32
    proj = xn @ w_in
    cuts = [int(i) for i in np.cumsum(HYB_SPLITS)[:-1]]
    gq, gk, gv, gr, glr, mq, mk, mv, mo, mi, mf = jnp.split(proj, cuts, axis=-1)
    log_a = jax.nn.log_sigmoid((glr @ w_gate2 + b_gate).astype(f32)) / GLA_TAU
    o_gla = gla_chunked(split_heads(gq, GLA_HEADS).astype(f32) * GLA_DK ** -0.5,
                        split_heads(gk, GLA_HEADS).astype(f32),
                        split_heads(gv, GLA_HEADS).astype(f32),
                        split_heads(log_a, GLA_HEADS))
    y_gla = rms_norm(o_gla.astype(xn.dtype).transpose(0, 2, 1, 3), gla_gain.reshape(GLA_HEADS, GLA_DV))
    y_gla = y_gla.reshape(b, s, GLA_V) * jax.nn.silu(gr)
    qk = jax.nn.silu(causal_conv(jnp.concatenate([mq, mk], axis=-1), conv_w))
    mq, mk = jnp.split(qk, 2, axis=-1)
    gates = (jnp.concatenate([mi, mf], axis=-1) + b_if).astype(f32).transpose(0, 2, 1)
    i_pre, f_pre = gates[:, :ML_HEADS], gates[:, ML_HEADS:]
    h_ml = mlstm_chunked(split_heads(mq, ML_HEADS).astype(f32),
                         split_heads(mk, ML_HEADS).astype(f32) * ML_DK ** -0.5,
                         split_heads(mv, ML_HEADS).astype(f32),
                         i_pre, jax.nn.log_sigmoid(f_pre))
    h_ml = h_ml.astype(xn.dtype).transpose(0, 2, 1, 3) * jax.nn.sigmoid(mo).reshape(b, s, ML_HEADS, ML_DV)
    y_ml = rms_norm(h_ml, ml_gain.reshape(ML_HEADS, ML_DV)).reshape(b, s, ML_V)
    return jnp.concatenate([y_gla, y_ml], axis=-1) @ w_out


def moba_attention(q, k, v):
    b, h, s, d = q.shape
    f32 = jnp.float32
    s_pad = -(-s // MOBA_BLOCK) * MOBA_BLOCK
    pad = ((0, 0), (0, 0), (0, s_pad - s), (0, 0))
    q, k, v = jnp.pad(q, pad), jnp.pad(k, pad), jnp.pad(v, pad)
    nb = s_pad // MOBA_BLOCK
    kb = k.reshape(b, h, nb, MOBA_BLOCK, d)
    vb = v.reshape(b, h, nb, MOBA_BLOCK, d)
    kmean = jnp.mean(kb.astype(f32), axis=3)
    pos = jnp.arange(s_pad, dtype=jnp.int32)
    qblk = pos // MOBA_BLOCK
    gate = jnp.einsum('bhtd,bhnd->bhtn', q.astype(f32), kmean)
    fully_past = jnp.arange(nb)[None, :] < qblk[:, None]
    gate = jnp.where(fully_past, gate, -jnp.inf)
    n_sel = min(MOBA_TOPK, nb)
    _, top = lax.top_k(gate, n_sel)
    idx = jnp.concatenate([top.astype(jnp.int32),
                           jnp.broadcast_to(qblk[:, None], (b, h, s_pad, 1))], axis=-1)
    valid = jnp.concatenate([jnp.arange(n_sel)[None, :] < qblk[:, None],
                             jnp.ones((s_pad, 1), bool)], axis=-1)
    slopes = alibi_slopes(h)[:, None, None, None]
    nq = s_pad // MOBA_QCHUNK
    q_c = jnp.moveaxis(q.reshape(b, h, nq, MOBA_QCHUNK, d), 2, 0)
    i_c = jnp.moveaxis(idx.reshape(b, h, nq, MOBA_QCHUNK, -1), 2, 0)
    t_c = pos.reshape(nq, MOBA_QCHUNK)
    v_c = valid.reshape(nq, MOBA_QCHUNK, -1)
    gather = jax.vmap(jax.vmap(lambda blocks, i: blocks[i]))

    def attend(inp):
        qq, ii, tt, vv = inp
        kg = gather(kb, ii)
        vg = gather(vb, ii)
        sc = jnp.einsum('bhqd,bhqrkd->bhqrk', qq, kg).astype(f32)
        kpos = ii[..., None] * MOBA_BLOCK + jnp.arange(MOBA_BLOCK, dtype=jnp.int32)
        dist = tt[:, None, None] - kpos
        allowed = vv[:, :, None] & (dist >= 0)
        sc = jnp.where(allowed, sc - slopes * dist.astype(f32), NEG)
        p = jax.nn.softmax(sc.reshape(*sc.shape[:3], -1), axis=-1).reshape(sc.shape)
        return jnp.einsum('bhqrk,bhqrkd->bhqd', p.astype(vg.dtype), vg)

    out = lax.map(attend, (q_c, i_c, t_c, v_c))
    return jnp.moveaxis(out, 0, 2).reshape(b, h, s_pad, d)[:, :, :s]


def moba_mixer(xn, w_qkv, q_gain, k_gain, w_o):
    q, k, v = jnp.split(xn @ w_qkv, 3, axis=-1)
    q = rms_norm(split_heads(q, MOBA_HEADS), q_gain) * MOBA_HD ** -0.5
    k = rms_norm(split_heads(k, MOBA_HEADS), k_gain)
    o = moba_attention(q, k, split_heads(v, MOBA_HEADS))
    return merge_heads(o) @ w_o


def swiglu(xn, wg, wu, wd):
    return (jax.nn.silu(xn @ wg) * (xn @ wu)) @ wd


def setup_inputs(seed: int = 0) -> dict:
    key = jax.random.key(seed)
    ks = jax.random.split(key, 24)

    def nrm(k, shape, scale):
        return jax.random.normal(k, shape, jnp.float32) * scale

    b_i = nrm(ks[8], (N_EVEN, ML_HEADS), 0.1)
    b_f = jnp.linspace(3.0, 6.0, ML_HEADS, dtype=jnp.float32)[None, :] + nrm(ks[9], (N_EVEN, ML_HEADS), 0.1)
    return {
        'x': nrm(ks[0], (BATCH, SEQ, D_MODEL), 1.0),
        'norm_mix': 1.0 + nrm(ks[1], (DEPTH, D_MODEL), 0.02),
        'norm_ffn': 1.0 + nrm(ks[2], (DEPTH, D_MODEL), 0.02),
        'hyb_w_in': nrm(ks[3], (N_EVEN, D_MODEL, HYB_IN), D_MODEL ** -0.5),
        'gla_w_gate2': nrm(ks[4], (N_EVEN, GLA_RANK, GLA_QK), GLA_RANK ** -0.5),
        'gla_b_gate': nrm(ks[5], (N_EVEN, GLA_QK), 0.1),
        'gla_norm': 1.0 + nrm(ks[6], (N_EVEN, GLA_V), 0.02),
        'ml_conv': nrm(ks[7], (N_EVEN, ML_CONV, 2 * ML_QK), ML_CONV ** -0.5),
        'ml_b_if': jnp.concatenate([b_i, b_f], axis=-1),
        'ml_norm': 1.0 + nrm(ks[10], (N_EVEN, ML_V), 0.02),
        'hyb_w_out': nrm(ks[11], (N_EVEN, GLA_V + ML_V, D_MODEL), (GLA_V + ML_V) ** -0.5),
        'moba_w_qkv': nrm(ks[12], (N_ODD, D_MODEL, 3 * D_MODEL), D_MODEL ** -0.5),
        'moba_q_norm': 1.0 + nrm(ks[13], (N_ODD, MOBA_HD), 0.02),
        'moba_k_norm': 1.0 + nrm(ks[14], (N_ODD, MOBA_HD), 0.02),
        'moba_w_o': nrm(ks[15], (N_ODD, D_MODEL, D_MODEL), D_MODEL ** -0.5),
        'ffn_w_gate': nrm(ks[16], (DEPTH, D_MODEL, D_FF), D_MODEL ** -0.5),
        'ffn_w_up': nrm(ks[17], (DEPTH, D_MODEL, D_FF), D_MODEL ** -0.5),
        'ffn_w_down': nrm(ks[18], (DEPTH, D_FF, D_MODEL), D_FF ** -0.5),
    }


def reference(x, norm_mix, norm_ffn, hyb_w_in, gla_w_gate2, gla_b_gate, gla_norm, ml_conv, ml_b_if,
              ml_norm, hyb_w_out, moba_w_qkv, moba_q_norm, moba_k_norm, moba_w_o,
              ffn_w_gate, ffn_w_up, ffn_w_down):
    for l in range(DEPTH):
        xn = rms_norm(x, norm_mix[l])
        j = l // 2
        if l % 2 == 0:
            x = x + hybrid_mixer(xn, hyb_w_in[j], gla_w_gate2[j], gla_b_gate[j], gla_norm[j],
                                 ml_conv[j], ml_b_if[j], ml_norm[j], hyb_w_out[j])
        else:
            x = x + moba_mixer(xn, moba_w_qkv[j], moba_q_norm[j], moba_k_norm[j], moba_w_o[j])
        xn = rms_norm(x, norm_ffn[l])
        x = x + swiglu(xn, ffn_w_gate[l], ffn_w_up[l], ffn_w_down[l])
    return x
```

```python
import contextlib
import numpy as np
import concourse.bass as bass
import concourse.mybir as mybir
from concourse.bass_utils import run_bass_kernel_spmd

F32 = mybir.dt.float32
BF16 = mybir.dt.bfloat16
AF = mybir.ActivationFunctionType
ALU = mybir.AluOpType
AX = mybir.AxisListType

D = 1024
DFF = 2816
NFF = DFF // 128
DEPTH = 4
HYB_IN = 3096
EPS = 1e-6
BIG = 30000.0
ENGS = ("pe", "act", "dve", "pool", "sp")
NDMASEM = 24


class Prog:
    def __init__(self, nc):
        self.nc = nc
        self.ops = []
        self.cnt = {e: 0 for e in ENGS}
        self.last_w = {}
        self.readers = {}
        self.ndma = {e: 0 for e in ENGS}
        self.last_on = {e: None for e in ENGS}
        self.recent_dma = {e: [] for e in ENGS}

    def _add(self, eng, fn, reads, writes, dma, extra=()):
        deps = set(extra)
        for b in reads:
            w = self.last_w.get(b)
            if w is not None:
                deps.add(w)
        for b in writes:
            w = self.last_w.get(b)
            if w is not None:
                deps.add(w)
            for r in self.readers.get(b, ()):
                deps.add(r)
        oid = len(self.ops)
        op = dict(id=oid, eng=eng, fn=fn, deps=deps, dma=dma, dma_idx=None)
        if dma:
            op["dma_idx"] = self.ndma[eng]
            self.ndma[eng] += 1
            self.recent_dma[eng].append(oid)
            if len(self.recent_dma[eng]) > NDMASEM:
                self.recent_dma[eng].pop(0)
        self.cnt[eng] += 1
        self.ops.append(op)
        self.last_on[eng] = oid
        for b in reads:
            self.readers.setdefault(b, []).append(oid)
        for b in writes:
            self.last_w[b] = oid
            self.readers[b] = []
        return oid

    def op(self, eng, fn, reads=(), writes=()):
        return self._add(eng, fn, tuple(reads), tuple(writes), False)

    def dma(self, eng, fn, reads=(), writes=()):
        return self._add(eng, fn, tuple(reads), tuple(writes), True)

    def barrier(self):
        deps = [o for o in self.last_on.values() if o is not None] + [o for q in self.recent_dma.values() for o in q]
        for e in ENGS:
            self._add(e, lambda eng: eng.nop(), (), (), False, extra=deps)
        self.last_w = {}
        self.readers = {}

    def emit(self, final_wait_eng="sp"):
        nc = self.nc
        ops = self.ops
        signaled = set()
        for op in ops:
            for d in op["deps"]:
                if not ops[d]["dma"]:
                    signaled.add(d)
        sigcount = {}
        run = {e: 0 for e in ENGS}
        for op in ops:
            if op["dma"]:
                continue
            if op["id"] in signaled:
                run[op["eng"]] += 1
                sigcount[op["id"]] = run[op["eng"]]
        by_eng = {e: [o for o in ops if o["eng"] == e] for e in ENGS}
        ndma = self.ndma
        with contextlib.ExitStack() as st:
            esem = {e: st.enter_context(nc.semaphore("s_" + e)) for e in ENGS}
            dqs = [e for e in ENGS if ndma[e] > 0]
            dsem = {q: [st.enter_context(nc.semaphore("d%s_%d" % (q, i))) for i in range(NDMASEM)] for q in dqs}
            block = st.enter_context(nc.Block())
            engobj = {"pe": "tensor", "act": "scalar", "dve": "vector", "pool": "gpsimd", "sp": "sync"}

            def body_for(e):
                def body(eng):
                    waited_e = {x: 0 for x in ENGS}
                    waited_d = {}
                    for op in by_eng[e]:
                        need_e = {}
                        need_d = {}
                        for d in op["deps"]:
                            dop = ops[d]
                            if dop["dma"]:
                                k = dop["dma_idx"]
                                s = (dop["eng"], k % NDMASEM)
                                need_d[s] = max(need_d.get(s, 0), 16 * (k // NDMASEM + 1))
                            else:
                                de = dop["eng"]
                                need_e[de] = max(need_e.get(de, 0), sigcount[d])
                        if op["dma"]:
                            k = op["dma_idx"]
                            if k >= NDMASEM:
                                s = (e, k % NDMASEM)
                                need_d[s] = max(need_d.get(s, 0), 16 * (k // NDMASEM))
                        for de, v in need_e.items():
                            if v > waited_e[de]:
                                eng.wait_ge(esem[de], v)
                                waited_e[de] = v
                        for ds, v in need_d.items():
                            if v > waited_d.get(ds, 0):
                                eng.wait_ge(dsem[ds[0]][ds[1]], v)
                                waited_d[ds] = v
                        ins = op["fn"](eng)
                        if op["dma"]:
                            ins.then_inc(dsem[e][op["dma_idx"] % NDMASEM], 16)
                        elif op["id"] in signaled:
                            ins.then_inc(esem[e], 1)
                    if e == final_wait_eng:
                        for q in dqs:
                            for s in range(NDMASEM):
                                n = (ndma[q] - s + NDMASEM - 1) // NDMASEM if ndma[q] > s else 0
                                if n > 0 and 16 * n > waited_d.get((q, s), 0):
                                    eng.wait_ge(dsem[q][s], 16 * n)
                return body

            for e in ENGS:
                if by_eng[e] or e == final_wait_eng:
                    getattr(block, engobj[e])(body_for(e))


def _layout(T):
    off = {}
    n = 0

    def add(name, w):
        nonlocal n
        off[name] = (n, w)
        n += w
    add("nm", DEPTH * 8)
    add("nf", DEPTH * 8)
    for j in range(2):
        add("gb%d" % j, 2)
        add("gn%d" % j, 4)
        add("cw%d" % j, 16)
        add("bif%d" % j, 1)
        add("mn%d" % j, 4)
        add("qn%d" % j, 1)
        add("kn%d" % j, 1)
    add("bpt", 8 * 34)
    add("pbias", 256)
    add("pmb", 256)
    add("identf", 128)
    add("scr", 8)
    return off, n


def _layout_b():
    off = {}
    n = 0
    for name, w in (("ones", 128), ("ident", 128), ("tri", 128), ("cm", 512), ("E", 2048), ("sel", 512)):
        off[name] = (n, w)
        n += w
    return off, n


def _pack_consts(T, inp):
    off, n = _layout(T)
    cp = np.zeros((128, n), np.float32)

    def put(name, arr):
        o, w = off[name]
        arr = np.asarray(arr, np.float32)
        cp[:arr.shape[0], o:o + arr.shape[1]] = arr

    def fm(v, nch):
        return np.asarray(v, np.float32).reshape(nch, 128).T
    put("nm", np.concatenate([fm(inp["norm_mix"][l], 8) for l in range(DEPTH)], axis=1))
    put("nf", np.concatenate([fm(inp["norm_ffn"][l], 8) for l in range(DEPTH)], axis=1))
    for j in range(2):
        put("gb%d" % j, fm(inp["gla_b_gate"][j], 2))
        put("gn%d" % j, fm(inp["gla_norm"][j], 4))
        cw = np.zeros((128, 16), np.float32)
        conv = np.asarray(inp["ml_conv"][j], np.float32)
        for pp in range(2):
            for qk in range(2):
                for tap in range(4):
                    cw[:, pp * 8 + qk * 4 + tap] = conv[tap, qk * 256 + pp * 128: qk * 256 + pp * 128 + 128]
        put("cw%d" % j, cw)
        put("bif%d" % j, np.asarray(inp["ml_b_if"][j], np.float32).reshape(8, 1))
        put("mn%d" % j, fm(inp["ml_norm"][j], 4))
        put("qn%d" % j, np.asarray(inp["moba_q_norm"][j], np.float32).reshape(128, 1))
        put("kn%d" % j, np.asarray(inp["moba_k_norm"][j], np.float32).reshape(128, 1))
    bpt = np.zeros((128, 8 * 34), np.float32)
    sl = np.arange(128, dtype=np.float32)
    for h in range(8):
        slope = 2.0 ** (-(h + 1))
        for ri in range(34):
            r = ri - 1
            bpt[:, h * 34 + ri] = -slope * (128.0 * r + 255.0 - sl)
    put("bpt", bpt)
    pb = np.zeros((128, 256), np.float32)
    pm = np.zeros((128, 256), np.float32)
    for qb in range(16):
        for nb in range(16):
            pb[:, qb * 16 + nb] = 0.0 if nb < qb else -1e30
            pm[:, qb * 16 + nb] = BIG if nb < qb else 0.0
    put("pbias", pb)
    put("pmb", pm)
    sel = np.zeros((128, 512), np.float32)
    for pp in range(2):
        for hh in range(2):
            sel[4 + 2 * pp + hh, (pp * 2 + 0) * 128 + 64 * hh:(pp * 2 + 0) * 128 + 64 * hh + 64] = 1.0
            sel[2 * pp + hh, (pp * 2 + 1) * 128 + 64 * hh:(pp * 2 + 1) * 128 + 64 * hh + 64] = 1.0
    put("identf", np.eye(128, dtype=np.float32))
    offb, nb_ = _layout_b()
    cb = np.zeros((128, nb_), np.float32)
    o, w = offb["ones"]; cb[:, o:o + w] = 1.0
    o, w = offb["ident"]; cb[:, o:o + w] = np.eye(128)
    o, w = offb["tri"]; cb[:, o:o + w] = np.triu(np.ones((128, 128)))
    o, w = offb["cm"]
    s_ = np.arange(128)[:, None]
    t_ = np.arange(256)[None, :]
    for jj in range(2):
        cb[:, o + jj * 256:o + (jj + 1) * 256] = np.where(t_ >= 128 * jj + s_, 0.0, -BIG)
    o, w = offb["E"]
    for jb in range(16):
        cb[jb, o + jb * 128:o + (jb + 1) * 128] = 1.0
        cb[16, o + jb * 128:o + (jb + 1) * 128] = 1.0
    o, w = offb["sel"]; cb[:, o:o + w] = sel
    al = np.zeros((8, T), np.float32)
    tl = np.arange(T) % 256
    for h in range(8):
        al[h] = (2.0 ** (-(h + 1))) * (255.0 - tl)
    return cp, cb, al


class K:
    def __init__(self, T, layers=(0, 1, 2, 3), dbg=None):
        self.T = T
        self.NT = T // 512
        self.layers = layers
        self.dbg = dbg
        nc = self.nc = bass.Bass("TRN2", target_bir_lowering=False)
        self.p = Prog(nc)
        self.off, self.ncp = _layout(T)
        self.offb, self.ncb = _layout_b()
        dt = nc.dram_tensor
        self.xT_in = dt("xT", [D, T], F32, kind="ExternalInput").ap()
        self.cp_in = dt("cpack", [128, self.ncp], F32, kind="ExternalInput").ap()
        self.cb_in = dt("cbf", [128, self.ncb], F32, kind="ExternalInput").ap()
        self.al_in = dt("alrow", [8, T], F32, kind="ExternalInput").ap()
        self.w_in = dt("hyb_w_in", [2, D, HYB_IN], F32, kind="ExternalInput").ap()
        self.w_g2 = dt("gla_w_gate2", [2, 16, 256], F32, kind="ExternalInput").ap()
        self.w_ho = dt("hyb_w_out", [2, D, D], F32, kind="ExternalInput").ap()
        self.w_qkv = dt("moba_w_qkv", [2, D, 3 * D], F32, kind="ExternalInput").ap()
        self.w_mo = dt("moba_w_o", [2, D, D], F32, kind="ExternalInput").ap()
        self.w_fg = dt("ffn_w_gate", [DEPTH, D, DFF], F32, kind="ExternalInput").ap()
        self.w_fu = dt("ffn_w_up", [DEPTH, D, DFF], F32, kind="ExternalInput").ap()
        self.w_fd = dt("ffn_w_down", [DEPTH, DFF, D], F32, kind="ExternalInput").ap()
        self.outT = dt("outT", [D, T], F32, kind="ExternalOutput").ap()
        self.xres = dt("xres", [D, T], F32, kind="Internal").ap()
        self.yT = dt("yTs", [D, T], BF16, kind="Internal").ap()

    def un(self, name):
        self._uid = getattr(self, "_uid", 0) + 1
        return "%s_%d" % (name, self._uid)

    def mm(self, out, lhsT, rhs, start, stop, r, w):
        self.p.op("pe", lambda e: e.matmul(out, lhsT=lhsT, rhs=rhs, start=start, stop=stop), r, w)

    def tr(self, out, in_, ident, r, w):
        self.p.op("pe", lambda e: e.transpose(out, in_, ident), r, w)

    def act(self, out, in_, func, r, w, bias=None, scale=None):
        kw = {}
        if bias is not None:
            kw["bias"] = bias
        if scale is not None:
            kw["scale"] = scale
        self.p.op("act", lambda e: e.activation(out=out, in_=in_, func=func, **kw), r, w)

    def tt(self, eng, out, in0, in1, op, r, w):
        self.p.op(eng, lambda e: e.tensor_tensor(out=out, in0=in0, in1=in1, op=op), r, w)

    def ts(self, eng, out, in0, s1, s2, op0, op1, r, w):
        if op1 is None:
            self.p.op(eng, lambda e: e.tensor_scalar(out=out, in0=in0, scalar1=s1, scalar2=None, op0=op0), r, w)
        else:
            self.p.op(eng, lambda e: e.tensor_scalar(out=out, in0=in0, scalar1=s1, scalar2=s2, op0=op0, op1=op1), r, w)

    def stt(self, out, in0, scalar, in1, op0, op1, r, w):
        self.p.op("dve", lambda e: e.scalar_tensor_tensor(out=out, in0=in0, scalar=scalar, in1=in1, op0=op0, op1=op1), r, w)

    def cp(self, eng, out, in_, r, w):
        if eng == "act":
            self.p.op("act", lambda e: e.copy(out=out, in_=in_), r, w)
        else:
            self.p.op(eng, lambda e: e.tensor_copy(out=out, in_=in_), r, w)

    def dma(self, eng, out, in_, r, w):
        self.p.dma(eng, lambda e: e.dma_start(out=out, in_=in_), r, w)

    def C(self, name, i=0, n=1):
        o, w = self.off[name]
        return self.CP[:, o + i:o + i + n]

    def CB_(self, name, i=0, n=None):
        o, w = self.offb[name]
        if n is None:
            n = w
        return self.CB[:, o + i:o + i + n]

    def rms_fm(self, xs, gcols, outs, W, nfeat, tag, rx, wx, scale_extra=None):
        nch = len(xs)
        sq = self.sq
        for c in range(nch):
            self.act(sq[:, c, :W], xs[c], AF.Square, rx, ["sq%d" % c])
        ps = self.ps[6]
        for c in range(nch):
            self.mm(ps[:, :W], self.CB_("ones"), sq[:, c, :W], c == 0, c == nch - 1, ["sq%d" % c, "CB"], ["ps6"])
        rs = self.rstd
        self.act(rs[:, :W], ps[:, :W], AF.Ln, ["ps6", "CP"], ["rstd"], bias=self.C("scr", 0), scale=1.0 / nfeat)
        self.act(rs[:, :W], rs[:, :W], AF.Exp, ["rstd"], ["rstd"], scale=-0.5)
        for c in range(nch):
            self.stt(outs[c], xs[c], gcols[c], rs[:, :W], ALU.mult, ALU.mult, rx + ["rstd", "CP"], wx[c])

    def setup(self, st):
        nc = self.nc
        sb = lambda name, shape, dt_: st.enter_context(nc.sbuf_tensor(self.un(name), shape, dt_))
        self.CP = sb("CP", [128, self.ncp], F32)
        self.CB = sb("CB", [128, self.ncb], BF16)
        self.ps = [st.enter_context(nc.psum_tensor("ps%d" % i, [128, 512], F32)) for i in range(7)]
        self.psb = st.enter_context(nc.psum_tensor("psb", [128, 1024], BF16))
        self.sq = sb("sq", [128, 8, 512], BF16)
        self.rstd = sb("rstd", [128, 512], F32)
        self.dma("sp", self.CP[:], self.cp_in, [], ["CP"])
        self.dma("pool", self.CB[:], self.cb_in, [], ["CB"])
        o, _ = self.off["scr"]
        self.p.op("dve", lambda e: e.memset(self.CP[:, o:o + 1], EPS), ["CP"], ["CP"])
        self.p.op("dve", lambda e: e.memset(self.CP[:, o + 1:o + 2], 1.0), ["CP"], ["CP"])
        for j in range(2):
            self.ts("dve", self.C("qn%d" % j), self.C("qn%d" % j), float(128 ** -0.5), None, ALU.mult, None, ["CP"], ["CP"])
            self.ts("dve", self.C("gb%d" % j, 0, 2), self.C("gb%d" % j, 0, 2), -1.0, None, ALU.mult, None, ["CP"], ["CP"])

    def phase_c(self, l, w_o_ap, last):
        nc, T = self.nc, self.T
        with contextlib.ExitStack() as st:
            sb = lambda name, shape, dt_: st.enter_context(nc.sbuf_tensor(self.un(name), shape, dt_))
            wo = sb("wo", [128, 8, D], BF16)
            wg = sb("wg", [128, 8, DFF], BF16)
            wu = sb("wu", [128, 8, DFF], BF16)
            wd = [sb("wd%d" % i, [128, NFF, 128], BF16) for i in range(2)]
            xt = [sb("xt%d" % i, [128, 8, 512], F32) for i in range(1)]
            yt = [sb("yt%d" % i, [128, 8, 512], BF16) for i in range(1)]
            xn2 = sb("xn2", [128, 8, 512], BF16)
            hT = sb("hT", [128, NFF, 512], BF16)
            sg = [sb("sg%d" % i, [128, 512], F32) for i in range(2)]
            self.dma("pool", wo[:], w_o_ap.rearrange("(k p) n -> p k n", p=128), [], ["wo"])
            for (wt, src, nm) in ((wg, self.w_fg[l], "wg"), (wu, self.w_fu[l], "wu")):
                v = src.rearrange("(k p) n -> p k n", p=128)
                for hlf in range(2):
                    self.dma("pool", wt[:, :, hlf * 1408:(hlf + 1) * 1408], v[:, :, hlf * 1408:(hlf + 1) * 1408], [], [nm + str(hlf)])
            wdv = self.w_fd[l].rearrange("(f p) n -> p f n", p=128)
            xv = self.xres.rearrange("(c p) t -> p c t", p=128)
            yv = self.yT.rearrange("(c p) t -> p c t", p=128)
            ov = (self.outT if last else self.xres).rearrange("(c p) t -> p c t", p=128)
            nwd = 0
            for tt in range(self.NT):
                s = 0
                t0 = tt * 512
                X, Y = xt[s], yt[s]
                self.dma("sp", X[:], xv[:, :, t0:t0 + 512], [], ["xt%d" % s])
                self.dma("sp", Y[:], yv[:, :, t0:t0 + 512], [], ["yt%d" % s])
                for m in range(8):
                    ps = self.ps[m % 2]
                    for k in range(8):
                        self.mm(ps[:], wo[:, k, m * 128:(m + 1) * 128], Y[:, k, :], k == 0, k == 7,
                                ["wo", "yt%d" % s], ["ps%d" % (m % 2)])
                    self.tt("dve", X[:, m, :], ps[:], X[:, m, :], ALU.add, ["ps%d" % (m % 2), "xt%d" % s], ["xt%d" % s])
                gc = [self.C("nf", l * 8 + c) for c in range(8)]
                self.rms_fm([X[:, c, :] for c in range(8)], gc, [xn2[:, c, :] for c in range(8)], 512, D,
                            "c", ["xt%d" % s], [["xn2"]] * 8)
                for f in range(NFF):
                    pg, pu = self.ps[2 + (f % 2)], self.ps[4 + (f % 2)]
                    hlf = "0" if f * 128 < 1408 else "1"
                    for k in range(8):
                        self.mm(pg[:], wg[:, k, f * 128:(f + 1) * 128], xn2[:, k, :], k == 0, k == 7,
                                ["wg" + hlf, "xn2"], ["ps%d" % (2 + f % 2)])
                    for k in range(8):
                        self.mm(pu[:], wu[:, k, f * 128:(f + 1) * 128], xn2[:, k, :], k == 0, k == 7,
                                ["wu" + hlf, "xn2"], ["ps%d" % (4 + f % 2)])
                    self.act(sg[f % 2][:], pg[:], AF.Silu, ["ps%d" % (2 + f % 2)], ["sg%d" % (f % 2)])
                    self.tt("dve", hT[:, f, :], sg[f % 2][:], pu[:], ALU.mult, ["sg%d" % (f % 2), "ps%d" % (4 + f % 2)], ["hT%d" % f])
                for m in range(8):
                    ws = nwd % 2
                    nwd += 1
                    self.dma("pool", wd[ws][:], wdv[:, :, m * 128:(m + 1) * 128], [], ["wd%d" % ws])
                    ps = self.ps[m % 2]
                    for f in range(NFF):
                        self.mm(ps[:], wd[ws][:, f, :], hT[:, f, :], f == 0, f == NFF - 1,
                                ["wd%d" % ws, "hT%d" % f], ["ps%d" % (m % 2)])
                    self.tt("dve", X[:, m, :], ps[:], X[:, m, :], ALU.add, ["ps%d" % (m % 2), "xt%d" % s], ["xt%d" % s])
                self.dma("sp", ov[:, :, t0:t0 + 512], X[:], ["xt%d" % s], ["xres_t%d" % tt] if not last else [])
        self.p.barrier()

    def phase_a(self, l, xnT, src, st):
        nc = self.nc
        xa = [st.enter_context(nc.sbuf_tensor(self.un("xa%d" % i), [128, 8, 512], F32)) for i in range(2)]
        xv = src.rearrange("(c p) t -> p c t", p=128)
        for tt in range(self.NT):
            s = tt % 2
            t0 = tt * 512
            self.dma("sp", xa[s][:], xv[:, :, t0:t0 + 512], [], ["xa%d" % s])
            gc = [self.C("nm", l * 8 + c) for c in range(8)]
            self.rms_fm([xa[s][:, c, :] for c in range(8)], gc, [xnT[:, c, t0:t0 + 512] for c in range(8)], 512, D,
                        "a", ["xa%d" % s], [["xnT%d" % tt]] * 8)

    def phase_moba(self, l, xnT, st):
        nc, T, NT = self.nc, self.T, self.NT
        j = l // 2
        NB = T // 256
        sb = lambda name, shape, dt_: st.enter_context(nc.sbuf_tensor(self.un(name), shape, dt_))
        wq = [sb("wq%d" % i, [128, 8, 384], BF16) for i in range(2)]
        qT = sb("qT", [128, T], BF16)
        kT = sb("kT", [128, T], BF16)
        V = sb("V", [128, T // 128, 128], BF16)
        kmf = sb("kmf", [128, 16], F32)
        kmb = sb("kmb", [128, 16], BF16)
        SELT = sb("SELT", [17, T], BF16)
        gm = sb("gm", [128, 16], F32)
        top8 = sb("top8", [128, 8], F32)
        selb = sb("selb", [128, 16], F32)
        PT = [sb("PT%d" % i, [128, 256], BF16) for i in range(3)]
        rden = sb("rden", [128, 256], F32)
        yh = [sb("yh%d" % i, [128, T], BF16) for i in range(2)]
        wv_ = self.w_qkv[j].rearrange("(k p) n -> p k n", p=128)
        yv = self.yT
        for h in range(8):
            hs = h % 2
            W = wq[hs]
            for part in range(3):
                self.dma("pool", W[:, :, part * 128:(part + 1) * 128], wv_[:, :, part * D + h * 128: part * D + (h + 1) * 128],
                         [], ["wq%d_%d" % (hs, part)])
            self.dma("pool", SELT[16:17, :], self.al_in[h:h + 1, :], [], ["SELTa"])
            for tt in range(NT):
                t0 = tt * 512
                for qi, (dst, gn) in enumerate(((qT, "qn%d" % j), (kT, "kn%d" % j))):
                    ps = self.ps[qi]
                    for k in range(8):
                        self.mm(ps[:], W[:, k, qi * 128:(qi + 1) * 128], xnT[:, k, t0:t0 + 512], k == 0, k == 7,
                                ["wq%d_%d" % (hs, qi), "xnT%d" % tt], ["ps%d" % qi])
                    self.rms_fm([ps[:]], [self.C(gn)], [dst[:, t0:t0 + 512]], 512, 128, "m", ["ps%d" % qi],
                                [["%sT%d" % ("qk"[qi], tt)]])
                for u in range(4):
                    ti = tt * 4 + u
                    ps = self.ps[2 + (ti % 2)]
                    for k in range(8):
                        self.mm(ps[:, :128], xnT[:, k, ti * 128:(ti + 1) * 128], W[:, k, 256:384], k == 0, k == 7,
                                ["wq%d_2" % hs, "xnT%d" % tt], ["ps%d" % (2 + ti % 2)])
                    self.cp("act", V[:, ti, :], ps[:, :128], ["ps%d" % (2 + ti % 2)], ["V%d" % (ti // 2)])
            self.p.op("dve", lambda e: e.tensor_reduce(out=kmf[:, :NB], in_=kT[:].rearrange("p (n s) -> p n s", s=256),
                                                      axis=AX.X, op=ALU.add),
                      ["kT%d" % tt for tt in range(NT)], ["kmf"])
            self.ts("dve", kmb[:, :NB], kmf[:, :NB], 1.0 / 256.0, None, ALU.mult, None, ["kmf"], ["kmb"])
            if NB < 16:
                self.p.op("dve", lambda e: e.memset(gm[:], -1e30), [], ["gm"])
            for qt in range(T // 128):
                qb = qt // 2
                ps = self.ps[2 + (qt % 2)]
                self.mm(ps[:, :NB], qT[:, qt * 128:(qt + 1) * 128], kmb[:, :NB], True, True,
                        ["qT%d" % (qt // 4), "kmb"], ["ps%d" % (2 + qt % 2)])
                self.tt("dve", gm[:, :NB], ps[:, :NB], self.C("pbias", qb * 16, NB), ALU.add, ["ps%d" % (2 + qt % 2), "CP"], ["gm"])
                self.p.op("dve", lambda e: e.max(out=top8[:], in_=gm[:]), ["gm"], ["top8"])
                self.ts("dve", selb[:], gm[:], top8[:, 2:3], 1.0, ALU.is_ge, ALU.subtract, ["gm", "top8"], ["selb"])
                self.tt("dve", selb[:], selb[:], self.C("pmb", qb * 16, 16), ALU.mult, ["selb", "CP"], ["selb"])
                pt = self.ps[4 + (qt % 2)]
                self.tr(pt[:16, :128], selb[:], self.C("identf", 0, 128), ["selb", "CP"], ["ps%d" % (4 + qt % 2)])
                self.cp("act", SELT[0:16, qt * 128:(qt + 1) * 128], pt[:16, :128], ["ps%d" % (4 + qt % 2)], ["SELT%d" % (qt // 2)])
            npt = 0
            for qb in range(NB):
                t0 = qb * 256
                pso, psd = self.ps[4], self.ps[5]
                nk = 2 * qb + 2
                for kt in range(nk):
                    pss = self.ps[kt % 2]
                    diag = kt >= 2 * qb
                    self.mm(pss[:, :256], kT[:, kt * 128:(kt + 1) * 128], qT[:, t0:t0 + 256], True, False,
                            ["kT%d" % (kt // 4), "qT%d" % (qb // 2)], ["ps%d" % (kt % 2)])
                    o, _ = self.offb["E"]
                    self.mm(pss[:, :256], self.CB[0:17, o + (kt // 2) * 128:o + (kt // 2 + 1) * 128], SELT[0:17, t0:t0 + 256],
                            False, not diag, ["CB", "SELT%d" % qb, "SELTa"], ["ps%d" % (kt % 2)])
                    if diag:
                        self.mm(pss[:, :256], self.CB_("ident"), self.CB_("cm", (kt - 2 * qb) * 256, 256), False, True,
                                ["CB"], ["ps%d" % (kt % 2)])
                    P = PT[npt % 3]
                    r = 2 * qb - kt
                    self.act(P[:], pss[:, :256], AF.Exp, ["ps%d" % (kt % 2), "CP"], ["PT%d" % (npt % 3)],
                             bias=self.C("bpt", h * 34 + r + 1))
                    self.mm(pso[:, :256], V[:, kt, :], P[:], kt == 0, kt == nk - 1, ["V%d" % (kt // 2), "PT%d" % (npt % 3)], ["ps4"])
                    self.mm(psd[:, :256], self.CB_("ones"), P[:], kt == 0, kt == nk - 1, ["CB", "PT%d" % (npt % 3)], ["ps5"])
                    npt += 1
                self.p.op("dve", lambda e: e.reciprocal(out=rden[:], in_=psd[:, :256]), ["ps5"], ["rden"])
                self.tt("dve", yh[hs][:, t0:t0 + 256], pso[:, :256], rden[:], ALU.mult, ["ps4", "rden"], ["yh%d" % hs])
            self.dma("sp", yv[h * 128:(h + 1) * 128, :], yh[hs][:], ["yh%d" % hs], ["yT"])

    def phase_hybrid(self, l, xnT, st):
        nc, T, NT = self.nc, self.T, self.NT
        j = l // 2
        NCH = T // 128
        sb = lambda name, shape, dt_: st.enter_context(nc.sbuf_tensor(self.un(name), shape, dt_))
        wq = sb("hwq", [128, 8, 128], BF16)
        wk = sb("hwk", [128, 8, 128], BF16)
        wv = sb("hwv", [128, 8, 256], BF16)
        wgt = sb("hwg", [128, 8, 256], BF16)
        wlr = sb("hwlr", [128, 8, 16], BF16)
        wif = sb("hwif", [128, 8, 8], BF16)
        wg2 = sb("hwg2", [16, 256], BF16)
        qT = sb("hqT", [128, T], BF16)
        kT = sb("hkT", [128, T], BF16)
        QR = [sb("hQR%d" % i, [128, 515], F32) for i in range(2)]
        ACC0 = sb("hACC0", [128, 512], F32)
        ACC = [ACC0, ACC0]
        V = sb("hV", [128, NCH, 256], BF16)
        GA = sb("hGA", [128, 2, 512], BF16)
        lrT = sb("hlrT", [16, T], BF16)
        GT8 = sb("hGT8", [8, T], BF16)
        LF8 = lrT[0:8, :]
        LFt = sb("hLFt", [8, 512], F32)
        Bg = sb("hBg", [128, T + 1], F32)
        NBt = sb("hNB", [128, T], F32)
        BC = sb("hBC", [128, NCH + 1], F32)
        NBC = sb("hNBC", [128, NCH + 1], F32)
        onesf = sb("honesf", [128, 512], BF16)
        E1 = [sb("hE1_%d" % i, [128, 128], F32) for i in range(2)]
        E2 = sb("hE2", [128, 128], F32)
        E3 = sb("hE3", [128, 128], F32)
        qc = [sb("hqc%d" % i, [128, 128], BF16) for i in range(2)]
        kc = [sb("hkc%d" % i, [128, 128], BF16) for i in range(2)]
        kh = sb("hkh", [128, 128], BF16)
        khT = [sb("hkhT%d" % i, [128, 128], BF16) for i in range(2)]
        AT = [sb("hAT%d" % i, [128, 128], BF16) for i in range(2)]
        S32 = sb("hS32", [128, 128], F32)
        Sb = sb("hSb", [128, 128], BF16)
        N32 = sb("hN32", [128, 128], F32)
        Nb = sb("hNb", [128, 128], BF16)
        OT = sb("hOT", [128, 2, 512], F32)
        DD = sb("hDD", [128, 2, 128], F32)
        yo = [sb("hyo%d" % i, [128, 2, 512], BF16) for i in range(2)]
        psb = self.psb
        win = self.w_in[j].rearrange("(k p) n -> p k n", p=128)
        yv = self.yT
        osel = self.offb["sel"][0]

        def load_w(dst, c0, n, nm):
            self.dma("pool", dst[:, :, :n], win[:, :, c0:c0 + n], [], [nm])

        self.p.op("dve", lambda e: e.memset(onesf[:], 1.0), [], ["onesf"])
        self.p.op("dve", lambda e: e.memset(Bg[:, 0:1], 0.0), [], ["Bg0"])
        load_w(wlr, 1536, 16, "hwlr")
        load_w(wif, 3088, 8, "hwif")
        self.dma("pool", wg2[:], self.w_g2[j], [], ["hwg2"])
        obif = self.off["bif%d" % j][0]
        for tt in range(NT):
            t0 = tt * 512
            ps = self.ps[0]
            for k in range(8):
                self.mm(ps[:16, :], wlr[:, k, :], xnT[:, k, t0:t0 + 512], k == 0, k == 7, ["hwlr", "xnT%d" % tt], ["ps0"])
            self.cp("act", lrT[:, t0:t0 + 512], ps[:16, :], ["ps0"], ["hlrT%d" % tt])

        def gate_rows():
            for tt in range(NT):
                t0 = tt * 512
                ps = self.ps[1]
                for k in range(8):
                    self.mm(ps[:8, :], wif[:, k, :], xnT[:, k, t0:t0 + 512], k == 0, k == 7, ["hwif", "xnT%d" % tt], ["ps1"])
                self.act(GT8[:, t0:t0 + 512], ps[:8, :], AF.Identity, ["ps1", "CP"], ["hGT8"], bias=self.CP[0:8, obif:obif + 1])
                self.act(LFt[:], ps[:8, :], AF.Identity, ["ps1", "CP"], ["hLFt"], bias=self.CP[0:8, obif:obif + 1])
                self.act(LFt[:], LFt[:], AF.Exp, ["hLFt"], ["hLFt"], scale=-1.0)
                self.act(LFt[:], LFt[:], AF.Ln, ["hLFt", "CP"], ["hLFt"], bias=self.CP[0:8, self.off["scr"][0] + 1:self.off["scr"][0] + 2])
                self.ts("dve", LF8[:, t0:t0 + 512], LFt[:], -1.0, None, ALU.mult, None, ["hLFt"], ["hLF8"])

        for pair in range(4):
            ml = pair >= 2
            pp = pair % 2
            if pair == 2:
                gate_rows()
            if not ml:
                cq, ck, cv, cg = 128 * pp, 256 + 128 * pp, 512 + 256 * pp, 1024 + 256 * pp
            else:
                cq, ck, cv, cg = 1552 + 128 * pp, 1808 + 128 * pp, 2064 + 256 * pp, 2576 + 256 * pp
            load_w(wq, cq, 128, "hwq")
            load_w(wk, ck, 128, "hwk")
            load_w(wv, cv, 256, "hwv")
            load_w(wgt, cg, 256, "hwg")
            gnm = ("mn%d" if ml else "gn%d") % j
            for qi, (wt, dst, nm) in enumerate(((wq, qT, "hwq"), (wk, kT, "hwk"))):
                dn = "h%sT" % "qk"[qi]
                for tt in range(NT):
                    t0 = tt * 512
                    s = tt % 2
                    ps = self.ps[tt % 2]
                    for k in range(8):
                        self.mm(ps[:], wt[:, k, :], xnT[:, k, t0:t0 + 512], k == 0, k == 7, [nm, "xnT%d" % tt], ["ps%d" % (tt % 2)])
                    if not ml:
                        self.cp("act", dst[:, t0:t0 + 512], ps[:], ["ps%d" % (tt % 2)], [dn])
                    else:
                        self.cp("act", QR[s][:, 3:515], ps[:], ["ps%d" % (tt % 2)], ["hQR%d" % s])
                        if tt == 0:
                            self.p.op("dve", lambda e, s=s: e.memset(QR[s][:, 0:3], 0.0), [], ["hQRh%d" % s])
                        else:
                            self.cp("dve", QR[s][:, 0:3], QR[1 - s][:, 512:515], ["hQR%d" % (1 - s)], ["hQRh%d" % s])
                        o = self.off["cw%d" % j][0] + pp * 8 + qi * 4
                        rd = ["hQR%d" % s, "hQRh%d" % s, "CP"]
                        self.ts("dve", ACC[s][:], QR[s][:, 0:512], self.CP[:, o:o + 1], None, ALU.mult, None, rd, ["hACC0"])
                        for tap in range(1, 4):
                            self.stt(ACC[s][:], QR[s][:, tap:tap + 512], self.CP[:, o + tap:o + tap + 1], ACC[s][:], ALU.mult, ALU.add,
                                     rd + ["hACC0"], ["hACC0"])
                        self.act(dst[:, t0:t0 + 512], ACC[s][:], AF.Silu, ["hACC0"], [dn])
            for tt in range(NT):
                t0 = tt * 512
                for u in range(4):
                    ti = tt * 4 + u
                    ps = self.ps[4 + (ti % 2)]
                    for k in range(8):
                        self.mm(ps[:, :256], xnT[:, k, ti * 128:(ti + 1) * 128], wv[:, k, :], k == 0, k == 7,
                                ["hwv", "xnT%d" % tt], ["ps%d" % (4 + ti % 2)])
                    self.cp("act", V[:, ti, :], ps[:, :256], ["ps%d" % (4 + ti % 2)], ["hV"])
            for tt in range(NT):
                t0 = tt * 512
                ps = self.ps[2]
                if not ml:
                    self.mm(ps[:], wg2[:, pp * 128:(pp + 1) * 128], lrT[:, t0:t0 + 512], True, True, ["hwg2", "hlrT%d" % tt], ["ps2"])
                    self.act(NBt[:, t0:t0 + 512], ps[:], AF.Exp, ["ps2", "CP"], ["hNB"], bias=self.C("gb%d" % j, pp), scale=-1.0)
                    self.act(NBt[:, t0:t0 + 512], NBt[:, t0:t0 + 512], AF.Ln, ["hNB", "CP"], ["hNB"], bias=self.C("scr", 1))
                    self.ts("dve", NBt[:, t0:t0 + 512], NBt[:, t0:t0 + 512], -1.0 / 16.0, None, ALU.mult, None, ["hNB"], ["hNB"])
                else:
                    o = osel + (pp * 2 + 0) * 128
                    self.mm(ps[:], self.CB[0:8, o:o + 128], LF8[:, t0:t0 + 512], True, True, ["CB", "hLF8"], ["ps2"])
                    self.cp("act", NBt[:, t0:t0 + 512], ps[:], ["ps2"], ["hNB"])
                ini = Bg[:, t0:t0 + 1]
                self.p.op("dve", lambda e, t0=t0, ini=ini: e.tensor_tensor_scan(out=Bg[:, 1 + t0:1 + t0 + 512], data0=onesf[:],
                                                                                   data1=NBt[:, t0:t0 + 512], initial=ini,
                                                                                   op0=ALU.mult, op1=ALU.add),
                          ["hNB", "onesf", "hBg", "Bg0"], ["hBg"])
            self.cp("dve", BC[:, 0:NCH], Bg[:, 0:T].rearrange("p (n s) -> p n s", s=128)[:, :, 0], ["hBg", "Bg0"], ["hBC"])
            self.cp("dve", BC[:, NCH:NCH + 1], Bg[:, T:T + 1], ["hBg", "Bg0"], ["hBC"])
            self.ts("dve", NBC[:, :], BC[:, :], -1.0, None, ALU.mult, None, ["hBC"], ["hNBC"])
            for tt in range(NT):
                t0 = tt * 512
                if not ml:
                    self.ts("dve", NBt[:, t0:t0 + 512], Bg[:, 1 + t0:1 + t0 + 512], -1.0, None, ALU.mult, None, ["hBg"], ["hNB"])
                else:
                    ps = self.ps[2]
                    o = osel + (pp * 2 + 1) * 128
                    self.mm(ps[:], self.CB[0:8, o:o + 128], GT8[:, t0:t0 + 512], True, True, ["CB", "hGT8"], ["ps2"])
                    self.tt("dve", NBt[:, t0:t0 + 512], ps[:], Bg[:, 1 + t0:1 + t0 + 512], ALU.subtract, ["ps2", "hBg"], ["hNB"])
            self.p.barrier()
            self.p.op("dve", lambda e: e.memset(S32[:], 0.0), [], ["hS32"])
            self.p.op("dve", lambda e: e.memset(Sb[:], 0.0), [], ["hSb"])
            if ml:
                self.p.op("dve", lambda e: e.memset(N32[:], 0.0), [], ["hN32"])
                self.p.op("dve", lambda e: e.memset(Nb[:], 0.0), [], ["hNb"])
            for ci in range(NCH):
                c0 = ci * 128
                s2 = ci % 2
                u = ci % 4
                sl = slice(c0, c0 + 128)
                e1 = E1[s2]
                self.act(e1[:], Bg[:, 1 + c0:1 + c0 + 128], AF.Exp, ["hBg", "hNBC"], ["hE1_%d" % s2], bias=NBC[:, ci:ci + 1])
                self.act(E2[:], NBt[:, sl], AF.Exp, ["hNB", "hBC"], ["hE2"], bias=BC[:, ci:ci + 1])
                self.act(E3[:], NBt[:, sl], AF.Exp, ["hNB", "hBC"], ["hE3"], bias=BC[:, ci + 1:ci + 2])
                self.stt(qc[s2][:], qT[:, sl], 0.125, e1[:], ALU.mult, ALU.mult, ["hqT", "hE1_%d" % s2], ["hqc%d" % s2])
                self.tt("pool", kc[s2][:], kT[:, sl], E2[:], ALU.mult, ["hkT", "hE2"], ["hkc%d" % s2])
                self.tt("pool", kh[:], kT[:, sl], E3[:], ALU.mult, ["hkT", "hE3"], ["hkh"])
                self.tr(psb[:, :128], kh[:], self.CB_("ident"), ["hkh", "CB"], ["psb"])
                self.cp("act", khT[s2][:], psb[:, :128], ["psb"], ["hkhT%d" % s2])
                psU = self.ps[5]
                for hh in range(2):
                    hp = slice(64 * hh, 64 * hh + 64)
                    psA, psO, psD = self.ps[hh], self.ps[2 + hh], self.ps[4]
                    nA, nO, nD = "ps%d" % hh, "ps%d" % (2 + hh), "ps4"
                    self.mm(psA[:, 0:128], kc[s2][hp, :], qc[s2][hp, :], True, True, ["hkc%d" % s2, "hqc%d" % s2], [nA])
                    self.tt("dve", AT[hh][:], psA[:, 0:128], self.CB_("tri"), ALU.mult, [nA, "CB"], ["hAT%d" % hh])
                    self.mm(psO[:, 0:128], V[:, ci, hh * 128:(hh + 1) * 128], AT[hh][:], True, False, ["hV", "hAT%d" % hh], [nO])
                    self.mm(psO[:, 0:128], Sb[hp, :], qc[s2][hp, :], False, True, ["hSb", "hqc%d" % s2], [nO])
                    if ml:
                        self.mm(psD[:, hh * 128:(hh + 1) * 128], self.CB_("ones"), AT[hh][:], True, False, ["CB", "hAT%d" % hh], [nD])
                        self.mm(psD[:, hh * 128:(hh + 1) * 128], Nb[hp, :], qc[s2][hp, :], False, True, ["hNb", "hqc%d" % s2], [nD])
                    self.mm(psU[hp, 0:128], khT[s2][:, hp], V[:, ci, hh * 128:(hh + 1) * 128], True, True, ["hkhT%d" % s2, "hV"], ["ps5"])
                    if ml:
                        self.mm(psU[hp, 128:256], khT[s2][:, hp], self.CB_("ones"), True, True, ["hkhT%d" % s2, "CB"], ["ps5"])
                    if not ml:
                        self.cp("act", OT[:, hh, u * 128:(u + 1) * 128], psO[:, 0:128], [nO], ["hOT"])
                    else:
                        self.act(DD[:, hh, :], psD[:, hh * 128:(hh + 1) * 128], AF.Abs, [nD], ["hDD%d" % hh])
                        self.ts("dve", DD[:, hh, :], DD[:, hh, :], 1.0, None, ALU.max, None, ["hDD%d" % hh], ["hDD%d" % hh])
                        self.p.op("dve", lambda e, hh=hh: e.reciprocal(out=DD[:, hh, :], in_=DD[:, hh, :]), ["hDD%d" % hh], ["hDD%d" % hh])
                        self.tt("dve", OT[:, hh, u * 128:(u + 1) * 128], psO[:, 0:128], DD[:, hh, :], ALU.mult, [nO, "hDD%d" % hh], ["hOT"])
                self.stt(S32[:], S32[:], e1[:, 127:128], psU[:, 0:128], ALU.mult, ALU.add, ["hS32", "hE1_%d" % s2, "ps5"], ["hS32"])
                self.cp("act", Sb[:], S32[:], ["hS32"], ["hSb"])
                if ml:
                    self.stt(N32[:], N32[:], e1[:, 127:128], psU[:, 128:256], ALU.mult, ALU.add, ["hN32", "hE1_%d" % s2, "ps5"], ["hN32"])
                    self.cp("act", Nb[:], N32[:], ["hN32"], ["hNb"])
                if u == 3:
                    tt = ci // 4
                    t0 = tt * 512
                    Y = yo[tt % 2]
                    for hh in range(2):
                        head = 2 * pp + hh
                        ps = self.ps[hh]
                        for k in range(8):
                            self.mm(ps[:], wgt[:, k, hh * 128:(hh + 1) * 128], xnT[:, k, t0:t0 + 512], k == 0, k == 7,
                                    ["hwg", "xnT%d" % tt], ["ps%d" % hh])
                        self.act(GA[:, hh, :], ps[:], AF.Sigmoid if ml else AF.Silu, ["ps%d" % hh], ["hGA%d" % hh])
                        if ml:
                            self.tt("pool", OT[:, hh, :], OT[:, hh, :], GA[:, hh, :], ALU.mult, ["hOT", "hGA%d" % hh], ["hOT"])
                            self.rms_fm([OT[:, hh, :]], [self.C(gnm, head)], [Y[:, hh, :]], 512, 128, "h", ["hOT"], [["hyo%d" % (tt % 2)]])
                        else:
                            self.rms_fm([OT[:, hh, :]], [self.C(gnm, head)], [OT[:, hh, :]], 512, 128, "h", ["hOT"], [["hOT"]])
                            self.tt("pool", Y[:, hh, :], OT[:, hh, :], GA[:, hh, :], ALU.mult, ["hOT", "hGA%d" % hh], ["hyo%d" % (tt % 2)])
                    r0 = (512 if ml else 0) + pp * 256
                    self.dma("sp", yv[r0:r0 + 256, t0:t0 + 512].rearrange("(h p) t -> p h t", p=128), Y[:], ["hyo%d" % (tt % 2)], ["yT"])
            self.p.barrier()


    def build(self):
        nc = self.nc
        with contextlib.ExitStack() as st:
            self.setup(st)
            first = True
            for li, l in enumerate(self.layers):
                src = self.xT_in if first else self.xres
                with contextlib.ExitStack() as st2:
                    xnT = st2.enter_context(nc.sbuf_tensor(self.un("xnT"), [128, 8, self.T], BF16))
                    with contextlib.ExitStack() as st3:
                        self.phase_a(l, xnT, src, st3)
                    self.p.barrier()
                    with contextlib.ExitStack() as st3:
                        if l % 2 == 0:
                            self.phase_hybrid(l, xnT, st3)
                        else:
                            self.phase_moba(l, xnT, st3)
                    self.p.barrier()
                if first and self.dbg != "y":
                    self.dma("sp", self.xres, self.xT_in, [], ["xres_all"])
                    self.p.barrier()
                first = False
                w_o = (self.w_ho if l % 2 == 0 else self.w_mo)[l // 2]
                if self.dbg == "y":
                    self.dma("pool", self.outT, self.yT, [], [])
                    break
                self.phase_c(l, w_o, last=(li == len(self.layers) - 1))
            self.p.emit()
        return nc


_CACHE = {}


def _get_nc(T):
    if T not in _CACHE:
        _CACHE[T] = K(T).build()
    return _CACHE[T]


def make_in_maps(inputs, T, ncores=8):
    cp, cb, al = _pack_consts(T, inputs)
    x = np.asarray(inputs["x"], np.float32)
    nb = x.shape[0]
    shared = {k: np.ascontiguousarray(np.asarray(inputs[k], np.float32)) for k in
              ("hyb_w_in", "gla_w_gate2", "hyb_w_out", "moba_w_qkv", "moba_w_o", "ffn_w_gate", "ffn_w_up", "ffn_w_down")}
    maps = []
    for c in range(ncores):
        b = c % nb
        m = dict(shared)
        m["xT"] = np.ascontiguousarray(x[b].T)
        m["cpack"] = cp
        m["cbf"] = cb
        m["alrow"] = al
        maps.append(m)
    return maps


def kernel(**inputs):
    x = np.asarray(inputs["x"])
    B, T, _ = x.shape
    nc = _get_nc(T)
    maps = make_in_maps(inputs, T)
    res = run_bass_kernel_spmd(nc, maps, core_ids=list(range(8)))
    out = np.stack([np.ascontiguousarray(res.results[b]["outT"].T) for b in range(B)], axis=0)
    return out.astype(np.float32)
```

```python
import contextlib
import numpy as np
import concourse.bass as bass
import concourse.mybir as mybir
from concourse.bass_utils import run_bass_kernel_spmd

F32 = mybir.dt.float32
BF16 = mybir.dt.bfloat16
AF = mybir.ActivationFunctionType
ALU = mybir.AluOpType
AX = mybir.AxisListType

D = 1024
DFF = 2816
NFF = DFF // 128
DEPTH = 4
HYB_IN = 3096
EPS = 1e-6
BIG = 30000.0
ENGS = ("pe", "act", "dve", "pool", "sp")
NDMASEM = 24


class Prog:
    def __init__(self, nc):
        self.nc = nc
        self.ops = []
        self.cnt = {e: 0 for e in ENGS}
        self.last_w = {}
        self.readers = {}
        self.ndma = {e: 0 for e in ENGS}
        self.last_on = {e: None for e in ENGS}
        self.recent_dma = {e: [] for e in ENGS}

    def _add(self, eng, fn, reads, writes, dma, extra=()):
        deps = set(extra)
        for b in reads:
            w = self.last_w.get(b)
            if w is not None:
                deps.add(w)
        for b in writes:
            w = self.last_w.get(b)
            if w is not None:
                deps.add(w)
            for r in self.readers.get(b, ()):
                deps.add(r)
        if eng == "pe" and not dma:
            deps = {d for d in deps if not (self.ops[d]["eng"] == "pe" and not self.ops[d]["dma"])}
        oid = len(self.ops)
        op = dict(id=oid, eng=eng, fn=fn, deps=deps, dma=dma, dma_idx=None)
        if dma:
            op["dma_idx"] = self.ndma[eng]
            self.ndma[eng] += 1
            self.recent_dma[eng].append(oid)
            if len(self.recent_dma[eng]) > NDMASEM:
                self.recent_dma[eng].pop(0)
        self.cnt[eng] += 1
        self.ops.append(op)
        self.last_on[eng] = oid
        for b in reads:
            self.readers.setdefault(b, []).append(oid)
        for b in writes:
            self.last_w[b] = oid
            self.readers[b] = []
        return oid

    def op(self, eng, fn, reads=(), writes=()):
        return self._add(eng, fn, tuple(reads), tuple(writes), False)

    def dma(self, eng, fn, reads=(), writes=()):
        return self._add(eng, fn, tuple(reads), tuple(writes), True)

    def barrier(self):
        deps = [o for o in self.last_on.values() if o is not None] + [o for q in self.recent_dma.values() for o in q]
        for e in ENGS:
            self._add(e, lambda eng: eng.nop(), (), (), False, extra=deps)
        self.last_w = {}
        self.readers = {}

    def emit(self, final_wait_eng="sp"):
        nc = self.nc
        ops = self.ops
        signaled = set()
        for op in ops:
            for d in op["deps"]:
                if not ops[d]["dma"]:
                    signaled.add(d)
        sigcount = {}
        run = {e: 0 for e in ENGS}
        for op in ops:
            if op["dma"]:
                continue
            if op["id"] in signaled:
                run[op["eng"]] += 1
                sigcount[op["id"]] = run[op["eng"]]
        by_eng = {e: [o for o in ops if o["eng"] == e] for e in ENGS}
        ndma = self.ndma
        with contextlib.ExitStack() as st:
            esem = {e: st.enter_context(nc.semaphore("s_" + e)) for e in ENGS}
            dqs = [e for e in ENGS if ndma[e] > 0]
            dsem = {q: [st.enter_context(nc.semaphore("d%s_%d" % (q, i))) for i in range(NDMASEM)] for q in dqs}
            block = st.enter_context(nc.Block())
            engobj = {"pe": "tensor", "act": "scalar", "dve": "vector", "pool": "gpsimd", "sp": "sync"}

            def body_for(e):
                def body(eng):
                    waited_e = {x: 0 for x in ENGS}
                    waited_d = {}
                    for op in by_eng[e]:
                        need_e = {}
                        need_d = {}
                        for d in op["deps"]:
                            dop = ops[d]
                            if dop["dma"]:
                                k = dop["dma_idx"]
                                s = (dop["eng"], k % NDMASEM)
                                need_d[s] = max(need_d.get(s, 0), 16 * (k // NDMASEM + 1))
                            else:
                                de = dop["eng"]
                                need_e[de] = max(need_e.get(de, 0), sigcount[d])
                        if op["dma"]:
                            k = op["dma_idx"]
                            if k >= NDMASEM:
                                s = (e, k % NDMASEM)
                                need_d[s] = max(need_d.get(s, 0), 16 * (k // NDMASEM))
                        for de, v in need_e.items():
                            if v > waited_e[de]:
                                eng.wait_ge(esem[de], v)
                                waited_e[de] = v
                        for ds, v in need_d.items():
                            if v > waited_d.get(ds, 0):
                                eng.wait_ge(dsem[ds[0]][ds[1]], v)
                                waited_d[ds] = v
                        ins = op["fn"](eng)
                        if op["dma"]:
                            ins.then_inc(dsem[e][op["dma_idx"] % NDMASEM], 16)
                        elif op["id"] in signaled:
                            ins.then_inc(esem[e], 1)
                    if e == final_wait_eng:
                        for q in dqs:
                            for s in range(NDMASEM):
                                n = (ndma[q] - s + NDMASEM - 1) // NDMASEM if ndma[q] > s else 0
                                if n > 0 and 16 * n > waited_d.get((q, s), 0):
                                    eng.wait_ge(dsem[q][s], 16 * n)
                return body

            for e in ENGS:
                if by_eng[e] or e == final_wait_eng:
                    getattr(block, engobj[e])(body_for(e))


def _layout(T):
    off = {}
    n = 0

    def add(name, w):
        nonlocal n
        off[name] = (n, w)
        n += w
    add("nm", DEPTH * 8)
    add("nf", DEPTH * 8)
    for j in range(2):
        add("gb%d" % j, 2)
        add("gn%d" % j, 4)
        add("cw%d" % j, 16)
        add("bif%d" % j, 1)
        add("mn%d" % j, 4)
        add("qn%d" % j, 1)
        add("kn%d" % j, 1)
    add("bpt", 8 * 34)
    add("pbias", 256)
    add("pmb", 256)
    add("identf", 128)
    add("scr", 8)
    return off, n


def _layout_b():
    off = {}
    n = 0
    for name, w in (("ones", 128), ("ident", 128), ("tri", 128), ("cm", 512), ("E", 2048), ("sel", 512)):
        off[name] = (n, w)
        n += w
    return off, n


def _pack_consts(T, inp):
    off, n = _layout(T)
    cp = np.zeros((128, n), np.float32)

    def put(name, arr):
        o, w = off[name]
        arr = np.asarray(arr, np.float32)
        cp[:arr.shape[0], o:o + arr.shape[1]] = arr

    def fm(v, nch):
        return np.asarray(v, np.float32).reshape(nch, 128).T
    put("nm", np.concatenate([fm(inp["norm_mix"][l], 8) for l in range(DEPTH)], axis=1))
    put("nf", np.concatenate([fm(inp["norm_ffn"][l], 8) for l in range(DEPTH)], axis=1))
    for j in range(2):
        put("gb%d" % j, fm(inp["gla_b_gate"][j], 2))
        put("gn%d" % j, fm(inp["gla_norm"][j], 4))
        cw = np.zeros((128, 16), np.float32)
        conv = np.asarray(inp["ml_conv"][j], np.float32)
        for pp in range(2):
            for qk in range(2):
                for tap in range(4):
                    cw[:, pp * 8 + qk * 4 + tap] = conv[tap, qk * 256 + pp * 128: qk * 256 + pp * 128 + 128]
        put("cw%d" % j, cw)
        put("bif%d" % j, np.asarray(inp["ml_b_if"][j], np.float32).reshape(8, 1))
        put("mn%d" % j, fm(inp["ml_norm"][j], 4))
        put("qn%d" % j, np.asarray(inp["moba_q_norm"][j], np.float32).reshape(128, 1))
        put("kn%d" % j, np.asarray(inp["moba_k_norm"][j], np.float32).reshape(128, 1))
    bpt = np.zeros((128, 8 * 34), np.float32)
    sl = np.arange(128, dtype=np.float32)
    for h in range(8):
        slope = 2.0 ** (-(h + 1))
        for ri in range(34):
            r = ri - 1
            bpt[:, h * 34 + ri] = -slope * (128.0 * r + 255.0 - sl)
    put("bpt", bpt)
    pb = np.zeros((128, 256), np.float32)
    pm = np.zeros((128, 256), np.float32)
    for qb in range(16):
        for nb in range(16):
            pb[:, qb * 16 + nb] = 0.0 if nb < qb else -1e30
            pm[:, qb * 16 + nb] = BIG if nb < qb else 0.0
    put("pbias", pb)
    put("pmb", pm)
    sel = np.zeros((128, 512), np.float32)
    for pp in range(2):
        for hh in range(2):
            sel[4 + 2 * pp + hh, (pp * 2 + 0) * 128 + 64 * hh:(pp * 2 + 0) * 128 + 64 * hh + 64] = 1.0
            sel[2 * pp + hh, (pp * 2 + 1) * 128 + 64 * hh:(pp * 2 + 1) * 128 + 64 * hh + 64] = 1.0
    put("identf", np.eye(128, dtype=np.float32))
    offb, nb_ = _layout_b()
    cb = np.zeros((128, nb_), np.float32)
    o, w = offb["ones"]; cb[:, o:o + w] = 1.0
    o, w = offb["ident"]; cb[:, o:o + w] = np.eye(128)
    o, w = offb["tri"]; cb[:, o:o + w] = np.triu(np.ones((128, 128)))
    o, w = offb["cm"]
    s_ = np.arange(128)[:, None]
    t_ = np.arange(256)[None, :]
    for jj in range(2):
        cb[:, o + jj * 256:o + (jj + 1) * 256] = np.where(t_ >= 128 * jj + s_, 0.0, -BIG)
    o, w = offb["E"]
    for jb in range(16):
        cb[jb, o + jb * 128:o + (jb + 1) * 128] = 1.0
        cb[16, o + jb * 128:o + (jb + 1) * 128] = 1.0
    o, w = offb["sel"]; cb[:, o:o + w] = sel
    al = np.zeros((8, T), np.float32)
    tl = np.arange(T) % 256
    for h in range(8):
        al[h] = (2.0 ** (-(h + 1))) * (255.0 - tl)
    return cp, cb, al


class K:
    def __init__(self, T, layers=(0, 1, 2, 3), dbg=None):
        self.T = T
        self.NT = T // 512
        self.layers = layers
        self.dbg = dbg
        nc = self.nc = bass.Bass("TRN2", target_bir_lowering=False)
        self.p = Prog(nc)
        self.off, self.ncp = _layout(T)
        self.offb, self.ncb = _layout_b()
        dt = nc.dram_tensor
        self.xT_in = dt("xT", [D, T], F32, kind="ExternalInput").ap()
        self.cp_in = dt("cpack", [128, self.ncp], F32, kind="ExternalInput").ap()
        self.cb_in = dt("cbf", [128, self.ncb], F32, kind="ExternalInput").ap()
        self.al_in = dt("alrow", [8, T], F32, kind="ExternalInput").ap()
        self.w_in = dt("hyb_w_in", [2, D, HYB_IN], F32, kind="ExternalInput").ap()
        self.w_g2 = dt("gla_w_gate2", [2, 16, 256], F32, kind="ExternalInput").ap()
        self.w_ho = dt("hyb_w_out", [2, D, D], F32, kind="ExternalInput").ap()
        self.w_qkv = dt("moba_w_qkv", [2, D, 3 * D], F32, kind="ExternalInput").ap()
        self.w_mo = dt("moba_w_o", [2, D, D], F32, kind="ExternalInput").ap()
        self.w_fg = dt("ffn_w_gate", [DEPTH, D, DFF], F32, kind="ExternalInput").ap()
        self.w_fu = dt("ffn_w_up", [DEPTH, D, DFF], F32, kind="ExternalInput").ap()
        self.w_fd = dt("ffn_w_down", [DEPTH, DFF, D], F32, kind="ExternalInput").ap()
        self.outT = dt("outT", [D, T], F32, kind="ExternalOutput").ap()
        self.xres = dt("xres", [D, T], F32, kind="Internal").ap()
        self.yT = dt("yTs", [D, T], BF16, kind="Internal").ap()

    def un(self, name):
        self._uid = getattr(self, "_uid", 0) + 1
        return "%s_%d" % (name, self._uid)

    def mm(self, out, lhsT, rhs, start, stop, r, w):
        self.p.op("pe", lambda e: e.matmul(out, lhsT=lhsT, rhs=rhs, start=start, stop=stop), r, w)

    def tr(self, out, in_, ident, r, w):
        self.p.op("pe", lambda e: e.transpose(out, in_, ident), r, w)

    def act(self, out, in_, func, r, w, bias=None, scale=None):
        kw = {}
        if bias is not None:
            kw["bias"] = bias
        if scale is not None:
            kw["scale"] = scale
        self.p.op("act", lambda e: e.activation(out=out, in_=in_, func=func, **kw), r, w)

    def tt(self, eng, out, in0, in1, op, r, w):
        self.p.op(eng, lambda e: e.tensor_tensor(out=out, in0=in0, in1=in1, op=op), r, w)

    def ts(self, eng, out, in0, s1, s2, op0, op1, r, w):
        if op1 is None:
            self.p.op(eng, lambda e: e.tensor_scalar(out=out, in0=in0, scalar1=s1, scalar2=None, op0=op0), r, w)
        else:
            self.p.op(eng, lambda e: e.tensor_scalar(out=out, in0=in0, scalar1=s1, scalar2=s2, op0=op0, op1=op1), r, w)

    def stt(self, out, in0, scalar, in1, op0, op1, r, w):
        self.p.op("dve", lambda e: e.scalar_tensor_tensor(out=out, in0=in0, scalar=scalar, in1=in1, op0=op0, op1=op1), r, w)

    def cp(self, eng, out, in_, r, w):
        if eng == "act":
            self.p.op("act", lambda e: e.copy(out=out, in_=in_), r, w)
        else:
            self.p.op(eng, lambda e: e.tensor_copy(out=out, in_=in_), r, w)

    def dma(self, eng, out, in_, r, w):
        self.p.dma(eng, lambda e: e.dma_start(out=out, in_=in_), r, w)

    def C(self, name, i=0, n=1):
        o, w = self.off[name]
        return self.CP[:, o + i:o + i + n]

    def CB_(self, name, i=0, n=None):
        o, w = self.offb[name]
        if n is None:
            n = w
        return self.CB[:, o + i:o + i + n]

    def rms_fm(self, xs, gcols, outs, W, nfeat, tag, rx, wx, scale_extra=None):
        nch = len(xs)
        sq = self.sq
        ps = self.ps[6]
        for c in range(nch):
            self.act(sq[:, c % 2, :W], xs[c], AF.Square, rx, ["sq%d" % (c % 2)])
            self.mm(ps[:, :W], self.CB_("ones"), sq[:, c % 2, :W], c == 0, c == nch - 1, ["sq%d" % (c % 2), "CB"], ["ps6"])
        rs = self.rstd
        self.act(rs[:, :W], ps[:, :W], AF.Ln, ["ps6", "CP"], ["rstd"], bias=self.C("scr", 0), scale=1.0 / nfeat)
        self.act(rs[:, :W], rs[:, :W], AF.Exp, ["rstd"], ["rstd"], scale=-0.5)
        for c in range(nch):
            self.stt(outs[c], xs[c], gcols[c], rs[:, :W], ALU.mult, ALU.mult, rx + ["rstd", "CP"], wx[c])

    def setup(self, st):
        nc = self.nc
        sb = lambda name, shape, dt_: st.enter_context(nc.sbuf_tensor(self.un(name), shape, dt_))
        self.CP = sb("CP", [128, self.ncp], F32)
        self.CB = sb("CB", [128, self.ncb], BF16)
        self.ps = [st.enter_context(nc.psum_tensor("ps%d" % i, [128, 512], F32)) for i in range(7)]
        self.psb = st.enter_context(nc.psum_tensor("psb", [128, 1024], BF16))
        self.sq = sb("sq", [128, 2, 512], BF16)
        self.rstd = sb("rstd", [128, 512], F32)
        self.dma("sp", self.CP[:], self.cp_in, [], ["CP"])
        self.dma("pool", self.CB[:], self.cb_in, [], ["CB"])
        o, _ = self.off["scr"]
        self.p.op("dve", lambda e: e.memset(self.CP[:, o:o + 1], EPS), ["CP"], ["CP"])
        self.p.op("dve", lambda e: e.memset(self.CP[:, o + 1:o + 2], 1.0), ["CP"], ["CP"])
        for j in range(2):
            self.ts("dve", self.C("qn%d" % j), self.C("qn%d" % j), float(128 ** -0.5), None, ALU.mult, None, ["CP"], ["CP"])
            self.ts("dve", self.C("gb%d" % j, 0, 2), self.C("gb%d" % j, 0, 2), -1.0, None, ALU.mult, None, ["CP"], ["CP"])

    def phase_c(self, l, w_o_ap, last):
        nc, T = self.nc, self.T
        HF = NFF // 2
        with contextlib.ExitStack() as st:
            sb = lambda name, shape, dt_: st.enter_context(nc.sbuf_tensor(self.un(name), shape, dt_))
            wo = sb("wo", [128, 8, D], BF16)
            wg = sb("wg", [128, 8, DFF], BF16)
            wu = sb("wu", [128, 8, DFF], BF16)
            wd = sb("wd", [128, NFF, D], BF16)
            xt = sb("xt", [128, 8, 512], F32)
            hT = sb("hT", [128, HF, 512], BF16)
            xn2 = sb("xn2", [128, 8, 512], BF16)
            sg = [sb("sg%d" % i, [128, 512], BF16) for i in range(2)]
            self.dma("pool", wo[:], w_o_ap.rearrange("(k p) n -> p k n", p=128), [], ["wo"])
            for (wt, src, nm) in ((wg, self.w_fg[l], "wg"), (wu, self.w_fu[l], "wu")):
                v = src.rearrange("(k p) n -> p k n", p=128)
                for hlf in range(2):
                    self.dma("pool", wt[:, :, hlf * 1408:(hlf + 1) * 1408], v[:, :, hlf * 1408:(hlf + 1) * 1408], [], [nm + str(hlf)])
            wdv = self.w_fd[l].rearrange("(f p) n -> p f n", p=128)
            for hlf in range(2):
                self.dma("pool", wd[:, hlf * HF:(hlf + 1) * HF, :], wdv[:, hlf * HF:(hlf + 1) * HF, :], [], ["wd%d" % hlf])
            xv = self.xres.rearrange("(c p) t -> p c t", p=128)
            yv = self.yT.rearrange("(c p) t -> p c t", p=128)
            ov = (self.outT if last else self.xres).rearrange("(c p) t -> p c t", p=128)
            hnames = ["hT%d" % f for f in range(HF)]
            for tt in range(self.NT):
                t0 = tt * 512
                X = xt
                Y = hT[:, 0:8, :]
                self.dma("sp", X[:], xv[:, :, t0:t0 + 512], [], ["xt0"])
                self.dma("sp", Y, yv[:, :, t0:t0 + 512], [], ["yt0"] + hnames)
                for m in range(8):
                    ps = self.ps[m % 2]
                    for k in range(8):
                        self.mm(ps[:], wo[:, k, m * 128:(m + 1) * 128], Y[:, k, :], k == 0, k == 7,
                                ["wo", "yt0"], ["ps%d" % (m % 2)])
                    self.tt("dve", X[:, m, :], ps[:], X[:, m, :], ALU.add, ["ps%d" % (m % 2), "xt0"], ["xt0"])
                gc = [self.C("nf", l * 8 + c) for c in range(8)]
                self.rms_fm([X[:, c, :] for c in range(8)], gc, [xn2[:, c, :] for c in range(8)], 512, D,
                            "c", ["xt0"], [["xn2"]] * 8)
                for hlf in range(2):
                    for fi in range(HF):
                        f = hlf * HF + fi
                        pg, pu = self.ps[2 + (f % 2)], self.ps[4 + (f % 2)]
                        wh = "0" if f * 128 < 1408 else "1"
                        for k in range(8):
                            self.mm(pg[:], wg[:, k, f * 128:(f + 1) * 128], xn2[:, k, :], k == 0, k == 7,
                                    ["wg" + wh, "xn2"], ["ps%d" % (2 + f % 2)])
                        for k in range(8):
                            self.mm(pu[:], wu[:, k, f * 128:(f + 1) * 128], xn2[:, k, :], k == 0, k == 7,
                                    ["wu" + wh, "xn2"], ["ps%d" % (4 + f % 2)])
                        self.act(sg[f % 2][:], pg[:], AF.Silu, ["ps%d" % (2 + f % 2)], ["sg%d" % (f % 2)])
                        self.tt("dve", hT[:, fi, :], sg[f % 2][:], pu[:], ALU.mult, ["sg%d" % (f % 2), "ps%d" % (4 + f % 2)],
                                ["hT%d" % fi, "yt0"])
                    for m in range(8):
                        ps = self.ps[m % 2]
                        for fi in range(HF):
                            f = hlf * HF + fi
                            self.mm(ps[:], wd[:, f, m * 128:(m + 1) * 128], hT[:, fi, :], fi == 0, fi == HF - 1,
                                    ["wd%d" % hlf, "hT%d" % fi], ["ps%d" % (m % 2)])
                        self.tt("dve", X[:, m, :], ps[:], X[:, m, :], ALU.add, ["ps%d" % (m % 2), "xt0"], ["xt0"])
                self.dma("sp", ov[:, :, t0:t0 + 512], X[:], ["xt0"], [])
        self.p.barrier()

    def phase_a(self, l, xnT, src, st):
        nc = self.nc
        xa = [st.enter_context(nc.sbuf_tensor(self.un("xa%d" % i), [128, 8, 512], F32)) for i in range(2)]
        xv = src.rearrange("(c p) t -> p c t", p=128)
        for tt in range(self.NT):
            s = tt % 2
            t0 = tt * 512
            self.dma("sp", xa[s][:], xv[:, :, t0:t0 + 512], [], ["xa%d" % s])
            gc = [self.C("nm", l * 8 + c) for c in range(8)]
            self.rms_fm([xa[s][:, c, :] for c in range(8)], gc, [xnT[:, c, t0:t0 + 512] for c in range(8)], 512, D,
                        "a", ["xa%d" % s], [["xnT%d" % tt]] * 8)

    def phase_moba(self, l, xnT, st):
        nc, T, NT = self.nc, self.T, self.NT
        j = l // 2
        NB = T // 256
        sb = lambda name, shape, dt_: st.enter_context(nc.sbuf_tensor(self.un(name), shape, dt_))
        wq = [sb("wq%d" % i, [128, 8, 384], BF16) for i in range(2)]
        qT = sb("qT", [128, T], BF16)
        kT = sb("kT", [128, T], BF16)
        V = sb("V", [128, T // 128, 128], BF16)
        kmf = sb("kmf", [128, 16], F32)
        kmb = sb("kmb", [128, 16], BF16)
        SELT = sb("SELT", [17, T], BF16)
        gm = sb("gm", [128, 16], F32)
        top8 = sb("top8", [128, 8], F32)
        selb = sb("selb", [128, 16], F32)
        PT = [sb("PT%d" % i, [128, 256], BF16) for i in range(3)]
        rden = sb("rden", [128, 256], F32)
        yh = [sb("yh%d" % i, [128, T], BF16) for i in range(2)]
        wv_ = self.w_qkv[j].rearrange("(k p) n -> p k n", p=128)
        yv = self.yT
        for h in range(8):
            hs = h % 2
            W = wq[hs]
            for part in range(3):
                self.dma("pool", W[:, :, part * 128:(part + 1) * 128], wv_[:, :, part * D + h * 128: part * D + (h + 1) * 128],
                         [], ["wq%d_%d" % (hs, part)])
            self.dma("pool", SELT[16:17, :], self.al_in[h:h + 1, :], [], ["SELTa"])
            for tt in range(NT):
                t0 = tt * 512
                for qi, (dst, gn) in enumerate(((qT, "qn%d" % j), (kT, "kn%d" % j))):
                    ps = self.ps[qi]
                    for k in range(8):
                        self.mm(ps[:], W[:, k, qi * 128:(qi + 1) * 128], xnT[:, k, t0:t0 + 512], k == 0, k == 7,
                                ["wq%d_%d" % (hs, qi), "xnT%d" % tt], ["ps%d" % qi])
                    self.rms_fm([ps[:]], [self.C(gn)], [dst[:, t0:t0 + 512]], 512, 128, "m", ["ps%d" % qi],
                                [["%sT%d" % ("qk"[qi], tt)]])
                for u in range(4):
                    ti = tt * 4 + u
                    ps = self.ps[2 + (ti % 2)]
                    for k in range(8):
                        self.mm(ps[:, :128], xnT[:, k, ti * 128:(ti + 1) * 128], W[:, k, 256:384], k == 0, k == 7,
                                ["wq%d_2" % hs, "xnT%d" % tt], ["ps%d" % (2 + ti % 2)])
                    self.cp("act", V[:, ti, :], ps[:, :128], ["ps%d" % (2 + ti % 2)], ["V%d" % (ti // 2)])
            self.p.op("dve", lambda e: e.tensor_reduce(out=kmf[:, :NB], in_=kT[:].rearrange("p (n s) -> p n s", s=256),
                                                      axis=AX.X, op=ALU.add),
                      ["kT%d" % tt for tt in range(NT)], ["kmf"])
            self.ts("dve", kmb[:, :NB], kmf[:, :NB], 1.0 / 256.0, None, ALU.mult, None, ["kmf"], ["kmb"])
            if NB < 16:
                self.p.op("dve", lambda e: e.memset(gm[:], -1e30), [], ["gm"])
            for qt in range(T // 128):
                qb = qt // 2
                ps = self.ps[2 + (qt % 2)]
                self.mm(ps[:, :NB], qT[:, qt * 128:(qt + 1) * 128], kmb[:, :NB], True, True,
                        ["qT%d" % (qt // 4), "kmb"], ["ps%d" % (2 + qt % 2)])
                self.tt("dve", gm[:, :NB], ps[:, :NB], self.C("pbias", qb * 16, NB), ALU.add, ["ps%d" % (2 + qt % 2), "CP"], ["gm"])
                self.p.op("dve", lambda e: e.max(out=top8[:], in_=gm[:]), ["gm"], ["top8"])
                self.ts("dve", selb[:], gm[:], top8[:, 2:3], 1.0, ALU.is_ge, ALU.subtract, ["gm", "top8"], ["selb"])
                self.tt("dve", selb[:], selb[:], self.C("pmb", qb * 16, 16), ALU.mult, ["selb", "CP"], ["selb"])
                pt = self.ps[4 + (qt % 2)]
                self.tr(pt[:16, :128], selb[:], self.C("identf", 0, 128), ["selb", "CP"], ["ps%d" % (4 + qt % 2)])
                self.cp("act", SELT[0:16, qt * 128:(qt + 1) * 128], pt[:16, :128], ["ps%d" % (4 + qt % 2)], ["SELT%d" % (qt // 2)])
            npt = 0
            for qb in range(NB):
                t0 = qb * 256
                pso, psd = self.ps[4], self.ps[5]
                nk = 2 * qb + 2
                for kt in range(nk):
                    pss = self.ps[kt % 2]
                    diag = kt >= 2 * qb
                    self.mm(pss[:, :256], kT[:, kt * 128:(kt + 1) * 128], qT[:, t0:t0 + 256], True, False,
                            ["kT%d" % (kt // 4), "qT%d" % (qb // 2)], ["ps%d" % (kt % 2)])
                    o, _ = self.offb["E"]
                    self.mm(pss[:, :256], self.CB[0:17, o + (kt // 2) * 128:o + (kt // 2 + 1) * 128], SELT[0:17, t0:t0 + 256],
                            False, not diag, ["CB", "SELT%d" % qb, "SELTa"], ["ps%d" % (kt % 2)])
                    if diag:
                        self.mm(pss[:, :256], self.CB_("ident"), self.CB_("cm", (kt - 2 * qb) * 256, 256), False, True,
                                ["CB"], ["ps%d" % (kt % 2)])
                    P = PT[npt % 3]
                    r = 2 * qb - kt
                    self.act(P[:], pss[:, :256], AF.Exp, ["ps%d" % (kt % 2), "CP"], ["PT%d" % (npt % 3)],
                             bias=self.C("bpt", h * 34 + r + 1))
                    self.mm(pso[:, :256], V[:, kt, :], P[:], kt == 0, kt == nk - 1, ["V%d" % (kt // 2), "PT%d" % (npt % 3)], ["ps4"])
                    self.mm(psd[:, :256], self.CB_("ones"), P[:], kt == 0, kt == nk - 1, ["CB", "PT%d" % (npt % 3)], ["ps5"])
                    npt += 1
                self.p.op("dve", lambda e: e.reciprocal(out=rden[:], in_=psd[:, :256]), ["ps5"], ["rden"])
                self.tt("dve", yh[hs][:, t0:t0 + 256], pso[:, :256], rden[:], ALU.mult, ["ps4", "rden"], ["yh%d" % hs])
            self.dma("sp", yv[h * 128:(h + 1) * 128, :], yh[hs][:], ["yh%d" % hs], ["yT"])

    def phase_hybrid(self, l, xnT, st):
        nc, T, NT = self.nc, self.T, self.NT
        j = l // 2
        NCH = T // 128
        sb = lambda name, shape, dt_: st.enter_context(nc.sbuf_tensor(self.un(name), shape, dt_))
        wq = sb("hwq", [128, 8, 128], BF16)
        wk = sb("hwk", [128, 8, 128], BF16)
        wv = sb("hwv", [128, 8, 256], BF16)
        wgt = sb("hwg", [128, 8, 256], BF16)
        wlr = sb("hwlr", [128, 8, 16], BF16)
        wif = sb("hwif", [128, 8, 8], BF16)
        wg2 = sb("hwg2", [16, 256], BF16)
        qT = sb("hqT", [128, T], BF16)
        kT = sb("hkT", [128, T], BF16)
        QR = [sb("hQR%d" % i, [128, 515], F32) for i in range(2)]
        ACC0 = sb("hACC0", [128, 512], F32)
        ACC = [ACC0, ACC0]
        V = sb("hV", [128, NCH, 256], BF16)
        GA = sb("hGA", [128, 2, 512], BF16)
        lrT = sb("hlrT", [16, T], BF16)
        GT8 = sb("hGT8", [8, T], BF16)
        LF8 = lrT[0:8, :]
        LFt = sb("hLFt", [8, 512], F32)
        Bg = sb("hBg", [128, T + 1], F32)
        NBt = sb("hNB", [128, T], F32)
        BC = sb("hBC", [128, NCH + 1], F32)
        NBC = sb("hNBC", [128, NCH + 1], F32)
        onesf = sb("honesf", [128, 512], BF16)
        E1 = [sb("hE1_%d" % i, [128, 128], F32) for i in range(2)]
        E2 = sb("hE2", [128, 128], F32)
        E3 = sb("hE3", [128, 128], F32)
        qc = [sb("hqc%d" % i, [128, 128], BF16) for i in range(2)]
        kc = [sb("hkc%d" % i, [128, 128], BF16) for i in range(2)]
        kh = sb("hkh", [128, 128], BF16)
        khT = [sb("hkhT%d" % i, [128, 128], BF16) for i in range(2)]
        AT = [sb("hAT%d" % i, [128, 128], BF16) for i in range(2)]
        S32 = sb("hS32", [128, 128], F32)
        Sb = sb("hSb", [128, 128], BF16)
        N32 = sb("hN32", [128, 128], F32)
        Nb = sb("hNb", [128, 128], BF16)
        OT = sb("hOT", [128, 2, 512], F32)
        DD = sb("hDD", [128, 2, 128], F32)
        yo = [sb("hyo%d" % i, [128, 2, 512], BF16) for i in range(2)]
        psb = self.psb
        win = self.w_in[j].rearrange("(k p) n -> p k n", p=128)
        yv = self.yT
        osel = self.offb["sel"][0]

        def load_w(dst, c0, n, nm):
            self.dma("pool", dst[:, :, :n], win[:, :, c0:c0 + n], [], [nm])

        self.p.op("dve", lambda e: e.memset(onesf[:], 1.0), [], ["onesf"])
        self.p.op("dve", lambda e: e.memset(Bg[:, 0:1], 0.0), [], ["Bg0"])
        load_w(wlr, 1536, 16, "hwlr")
        load_w(wif, 3088, 8, "hwif")
        self.dma("pool", wg2[:], self.w_g2[j], [], ["hwg2"])
        obif = self.off["bif%d" % j][0]
        for tt in range(NT):
            t0 = tt * 512
            ps = self.ps[0]
            for k in range(8):
                self.mm(ps[:16, :], wlr[:, k, :], xnT[:, k, t0:t0 + 512], k == 0, k == 7, ["hwlr", "xnT%d" % tt], ["ps0"])
            self.cp("act", lrT[:, t0:t0 + 512], ps[:16, :], ["ps0"], ["hlrT%d" % tt])

        def gate_rows():
            for tt in range(NT):
                t0 = tt * 512
                ps = self.ps[1]
                for k in range(8):
                    self.mm(ps[:8, :], wif[:, k, :], xnT[:, k, t0:t0 + 512], k == 0, k == 7, ["hwif", "xnT%d" % tt], ["ps1"])
                self.act(GT8[:, t0:t0 + 512], ps[:8, :], AF.Identity, ["ps1", "CP"], ["hGT8"], bias=self.CP[0:8, obif:obif + 1])
                self.act(LFt[:], ps[:8, :], AF.Identity, ["ps1", "CP"], ["hLFt"], bias=self.CP[0:8, obif:obif + 1])
                self.act(LFt[:], LFt[:], AF.Exp, ["hLFt"], ["hLFt"], scale=-1.0)
                self.act(LFt[:], LFt[:], AF.Ln, ["hLFt", "CP"], ["hLFt"], bias=self.CP[0:8, self.off["scr"][0] + 1:self.off["scr"][0] + 2])
                self.ts("dve", LF8[:, t0:t0 + 512], LFt[:], -1.0, None, ALU.mult, None, ["hLFt"], ["hLF8"])

        for pair in range(4):
            ml = pair >= 2
            pp = pair % 2
            if pair == 2:
                gate_rows()
            if not ml:
                cq, ck, cv, cg = 128 * pp, 256 + 128 * pp, 512 + 256 * pp, 1024 + 256 * pp
            else:
                cq, ck, cv, cg = 1552 + 128 * pp, 1808 + 128 * pp, 2064 + 256 * pp, 2576 + 256 * pp
            load_w(wq, cq, 128, "hwq")
            load_w(wk, ck, 128, "hwk")
            load_w(wv, cv, 256, "hwv")
            load_w(wgt, cg, 256, "hwg")
            gnm = ("mn%d" if ml else "gn%d") % j
            for qi, (wt, dst, nm) in enumerate(((wq, qT, "hwq"), (wk, kT, "hwk"))):
                dn = "h%sT" % "qk"[qi]
                for tt in range(NT):
                    t0 = tt * 512
                    s = tt % 2
                    ps = self.ps[tt % 2]
                    for k in range(8):
                        self.mm(ps[:], wt[:, k, :], xnT[:, k, t0:t0 + 512], k == 0, k == 7, [nm, "xnT%d" % tt], ["ps%d" % (tt % 2)])
                    if not ml:
                        self.cp("act", dst[:, t0:t0 + 512], ps[:], ["ps%d" % (tt % 2)], [dn])
                    else:
                        self.cp("act", QR[s][:, 3:515], ps[:], ["ps%d" % (tt % 2)], ["hQR%d" % s])
                        if tt == 0:
                            self.p.op("dve", lambda e, s=s: e.memset(QR[s][:, 0:3], 0.0), [], ["hQRh%d" % s])
                        else:
                            self.cp("dve", QR[s][:, 0:3], QR[1 - s][:, 512:515], ["hQR%d" % (1 - s)], ["hQRh%d" % s])
                        o = self.off["cw%d" % j][0] + pp * 8 + qi * 4
                        rd = ["hQR%d" % s, "hQRh%d" % s, "CP"]
                        self.ts("dve", ACC[s][:], QR[s][:, 0:512], self.CP[:, o:o + 1], None, ALU.mult, None, rd, ["hACC0"])
                        for tap in range(1, 4):
                            self.stt(ACC[s][:], QR[s][:, tap:tap + 512], self.CP[:, o + tap:o + tap + 1], ACC[s][:], ALU.mult, ALU.add,
                                     rd + ["hACC0"], ["hACC0"])
                        self.act(dst[:, t0:t0 + 512], ACC[s][:], AF.Silu, ["hACC0"], [dn])
            for tt in range(NT):
                t0 = tt * 512
                for u in range(4):
                    ti = tt * 4 + u
                    ps = self.ps[4 + (ti % 2)]
                    for k in range(8):
                        self.mm(ps[:, :256], xnT[:, k, ti * 128:(ti + 1) * 128], wv[:, k, :], k == 0, k == 7,
                                ["hwv", "xnT%d" % tt], ["ps%d" % (4 + ti % 2)])
                    self.cp("act", V[:, ti, :], ps[:, :256], ["ps%d" % (4 + ti % 2)], ["hV"])
            for tt in range(NT):
                t0 = tt * 512
                ps = self.ps[2]
                if not ml:
                    self.mm(ps[:], wg2[:, pp * 128:(pp + 1) * 128], lrT[:, t0:t0 + 512], True, True, ["hwg2", "hlrT%d" % tt], ["ps2"])
                    self.act(NBt[:, t0:t0 + 512], ps[:], AF.Exp, ["ps2", "CP"], ["hNB"], bias=self.C("gb%d" % j, pp), scale=-1.0)
                    self.act(NBt[:, t0:t0 + 512], NBt[:, t0:t0 + 512], AF.Ln, ["hNB", "CP"], ["hNB"], bias=self.C("scr", 1))
                    self.ts("dve", NBt[:, t0:t0 + 512], NBt[:, t0:t0 + 512], -1.0 / 16.0, None, ALU.mult, None, ["hNB"], ["hNB"])
                else:
                    o = osel + (pp * 2 + 0) * 128
                    self.mm(ps[:], self.CB[0:8, o:o + 128], LF8[:, t0:t0 + 512], True, True, ["CB", "hLF8"], ["ps2"])
                    self.cp("act", NBt[:, t0:t0 + 512], ps[:], ["ps2"], ["hNB"])
                ini = Bg[:, t0:t0 + 1]
                self.p.op("dve", lambda e, t0=t0, ini=ini: e.tensor_tensor_scan(out=Bg[:, 1 + t0:1 + t0 + 512], data0=onesf[:],
                                                                                   data1=NBt[:, t0:t0 + 512], initial=ini,
                                                                                   op0=ALU.mult, op1=ALU.add),
                          ["hNB", "onesf", "hBg", "Bg0"], ["hBg"])
            self.cp("dve", BC[:, 0:NCH], Bg[:, 0:T].rearrange("p (n s) -> p n s", s=128)[:, :, 0], ["hBg", "Bg0"], ["hBC"])
            self.cp("dve", BC[:, NCH:NCH + 1], Bg[:, T:T + 1], ["hBg", "Bg0"], ["hBC"])
            self.ts("dve", NBC[:, :], BC[:, :], -1.0, None, ALU.mult, None, ["hBC"], ["hNBC"])
            for tt in range(NT):
                t0 = tt * 512
                if not ml:
                    self.ts("dve", NBt[:, t0:t0 + 512], Bg[:, 1 + t0:1 + t0 + 512], -1.0, None, ALU.mult, None, ["hBg"], ["hNB"])
                else:
                    ps = self.ps[2]
                    o = osel + (pp * 2 + 1) * 128
                    self.mm(ps[:], self.CB[0:8, o:o + 128], GT8[:, t0:t0 + 512], True, True, ["CB", "hGT8"], ["ps2"])
                    self.tt("dve", NBt[:, t0:t0 + 512], ps[:], Bg[:, 1 + t0:1 + t0 + 512], ALU.subtract, ["ps2", "hBg"], ["hNB"])
            self.p.barrier()
            self.p.op("dve", lambda e: e.memset(S32[:], 0.0), [], ["hS32"])
            self.p.op("dve", lambda e: e.memset(Sb[:], 0.0), [], ["hSb"])
            if ml:
                self.p.op("dve", lambda e: e.memset(N32[:], 0.0), [], ["hN32"])
                self.p.op("dve", lambda e: e.memset(Nb[:], 0.0), [], ["hNb"])
            for ci in range(NCH):
                c0 = ci * 128
                s2 = ci % 2
                u = ci % 4
                sl = slice(c0, c0 + 128)
                e1 = E1[s2]
                self.act(e1[:], Bg[:, 1 + c0:1 + c0 + 128], AF.Exp, ["hBg", "hNBC"], ["hE1_%d" % s2], bias=NBC[:, ci:ci + 1])
                self.act(E2[:], NBt[:, sl], AF.Exp, ["hNB", "hBC"], ["hE2"], bias=BC[:, ci:ci + 1])
                self.act(E3[:], NBt[:, sl], AF.Exp, ["hNB", "hBC"], ["hE3"], bias=BC[:, ci + 1:ci + 2])
                self.stt(qc[s2][:], qT[:, sl], 0.125, e1[:], ALU.mult, ALU.mult, ["hqT", "hE1_%d" % s2], ["hqc%d" % s2])
                self.tt("pool", kc[s2][:], kT[:, sl], E2[:], ALU.mult, ["hkT", "hE2"], ["hkc%d" % s2])
                self.tt("pool", kh[:], kT[:, sl], E3[:], ALU.mult, ["hkT", "hE3"], ["hkh"])
                self.tr(psb[:, :128], kh[:], self.CB_("ident"), ["hkh", "CB"], ["psb"])
                self.cp("act", khT[s2][:], psb[:, :128], ["psb"], ["hkhT%d" % s2])
                psU = self.ps[5]
                for hh in range(2):
                    hp = slice(64 * hh, 64 * hh + 64)
                    psA, psO, psD = self.ps[hh], self.ps[2 + hh], self.ps[4]
                    nA, nO, nD = "ps%d" % hh, "ps%d" % (2 + hh), "ps4"
                    self.mm(psA[:, 0:128], kc[s2][hp, :], qc[s2][hp, :], True, True, ["hkc%d" % s2, "hqc%d" % s2], [nA])
                    self.tt("dve", AT[hh][:], psA[:, 0:128], self.CB_("tri"), ALU.mult, [nA, "CB"], ["hAT%d" % hh])
                    self.mm(psO[:, 0:128], V[:, ci, hh * 128:(hh + 1) * 128], AT[hh][:], True, False, ["hV", "hAT%d" % hh], [nO])
                    self.mm(psO[:, 0:128], Sb[hp, :], qc[s2][hp, :], False, True, ["hSb", "hqc%d" % s2], [nO])
                    if ml:
                        self.mm(psD[:, hh * 128:(hh + 1) * 128], self.CB_("ones"), AT[hh][:], True, False, ["CB", "hAT%d" % hh], [nD])
                        self.mm(psD[:, hh * 128:(hh + 1) * 128], Nb[hp, :], qc[s2][hp, :], False, True, ["hNb", "hqc%d" % s2], [nD])
                    self.mm(psU[hp, 0:128], khT[s2][:, hp], V[:, ci, hh * 128:(hh + 1) * 128], True, True, ["hkhT%d" % s2, "hV"], ["ps5"])
                    if ml:
                        self.mm(psU[hp, 128:256], khT[s2][:, hp], self.CB_("ones"), True, True, ["hkhT%d" % s2, "CB"], ["ps5"])
                    if not ml:
                        self.cp("act", OT[:, hh, u * 128:(u + 1) * 128], psO[:, 0:128], [nO], ["hOT"])
                    else:
                        self.act(DD[:, hh, :], psD[:, hh * 128:(hh + 1) * 128], AF.Abs, [nD], ["hDD%d" % hh])
                        self.ts("dve", DD[:, hh, :], DD[:, hh, :], 1.0, None, ALU.max, None, ["hDD%d" % hh], ["hDD%d" % hh])
                        self.p.op("dve", lambda e, hh=hh: e.reciprocal(out=DD[:, hh, :], in_=DD[:, hh, :]), ["hDD%d" % hh], ["hDD%d" % hh])
                        self.tt("dve", OT[:, hh, u * 128:(u + 1) * 128], psO[:, 0:128], DD[:, hh, :], ALU.mult, [nO, "hDD%d" % hh], ["hOT"])
                self.stt(S32[:], S32[:], e1[:, 127:128], psU[:, 0:128], ALU.mult, ALU.add, ["hS32", "hE1_%d" % s2, "ps5"], ["hS32"])
                self.cp("act", Sb[:], S32[:], ["hS32"], ["hSb"])
                if ml:
                    self.stt(N32[:], N32[:], e1[:, 127:128], psU[:, 128:256], ALU.mult, ALU.add, ["hN32", "hE1_%d" % s2, "ps5"], ["hN32"])
                    self.cp("act", Nb[:], N32[:], ["hN32"], ["hNb"])
                if u == 3:
                    tt = ci // 4
                    t0 = tt * 512
                    Y = yo[tt % 2]
                    for hh in range(2):
                        head = 2 * pp + hh
                        ps = self.ps[hh]
                        for k in range(8):
                            self.mm(ps[:], wgt[:, k, hh * 128:(hh + 1) * 128], xnT[:, k, t0:t0 + 512], k == 0, k == 7,
                                    ["hwg", "xnT%d" % tt], ["ps%d" % hh])
                        self.act(GA[:, hh, :], ps[:], AF.Sigmoid if ml else AF.Silu, ["ps%d" % hh], ["hGA%d" % hh])
                        if ml:
                            self.tt("pool", OT[:, hh, :], OT[:, hh, :], GA[:, hh, :], ALU.mult, ["hOT", "hGA%d" % hh], ["hOT"])
                            self.rms_fm([OT[:, hh, :]], [self.C(gnm, head)], [Y[:, hh, :]], 512, 128, "h", ["hOT"], [["hyo%d" % (tt % 2)]])
                        else:
                            self.rms_fm([OT[:, hh, :]], [self.C(gnm, head)], [OT[:, hh, :]], 512, 128, "h", ["hOT"], [["hOT"]])
                            self.tt("pool", Y[:, hh, :], OT[:, hh, :], GA[:, hh, :], ALU.mult, ["hOT", "hGA%d" % hh], ["hyo%d" % (tt % 2)])
                    r0 = (512 if ml else 0) + pp * 256
                    self.dma("sp", yv[r0:r0 + 256, t0:t0 + 512].rearrange("(h p) t -> p h t", p=128), Y[:], ["hyo%d" % (tt % 2)], ["yT"])
            self.p.barrier()


    def build(self):
        nc = self.nc
        with contextlib.ExitStack() as st:
            self.setup(st)
            first = True
            for li, l in enumerate(self.layers):
                src = self.xT_in if first else self.xres
                with contextlib.ExitStack() as st2:
                    xnT = st2.enter_context(nc.sbuf_tensor(self.un("xnT"), [128, 8, self.T], BF16))
                    with contextlib.ExitStack() as st3:
                        self.phase_a(l, xnT, src, st3)
                    self.p.barrier()
                    with contextlib.ExitStack() as st3:
                        if l % 2 == 0:
                            self.phase_hybrid(l, xnT, st3)
                        else:
                            self.phase_moba(l, xnT, st3)
                    self.p.barrier()
                if first and self.dbg != "y":
                    self.dma("sp", self.xres, self.xT_in, [], ["xres_all"])
                    self.p.barrier()
                first = False
                w_o = (self.w_ho if l % 2 == 0 else self.w_mo)[l // 2]
                if self.dbg == "y":
                    self.dma("pool", self.outT, self.yT, [], [])
                    break
                self.phase_c(l, w_o, last=(li == len(self.layers) - 1))
            self.p.emit()
        return nc


_CACHE = {}


def _get_nc(T):
    if T not in _CACHE:
        _CACHE[T] = K(T).build()
    return _CACHE[T]


def make_in_maps(inputs, T, ncores=8):
    cp, cb, al = _pack_consts(T, inputs)
    x = np.asarray(inputs["x"], np.float32)
    nb = x.shape[0]
    shared = {k: np.ascontiguousarray(np.asarray(inputs[k], np.float32)) for k in
              ("hyb_w_in", "gla_w_gate2", "hyb_w_out", "moba_w_qkv", "moba_w_o", "ffn_w_gate", "ffn_w_up", "ffn_w_down")}
    maps = []
    for c in range(ncores):
        b = c % nb
        m = dict(shared)
        m["xT"] = np.ascontiguousarray(x[b].T)
        m["cpack"] = cp
        m["cbf"] = cb
        m["alrow"] = al
        maps.append(m)
    return maps


def kernel(**inputs):
    x = np.asarray(inputs["x"])
    B, T, _ = x.shape
    nc = _get_nc(T)
    maps = make_in_maps(inputs, T)
    res = run_bass_kernel_spmd(nc, maps, core_ids=list(range(8)))
    out = np.stack([np.ascontiguousarray(res.results[b]["outT"].T) for b in range(B)], axis=0)
    return out.astype(np.float32)
```

```python
import contextlib
import numpy as np
import concourse.bass as bass
import concourse.mybir as mybir
from concourse.bass_utils import run_bass_kernel_spmd

F32 = mybir.dt.float32
BF16 = mybir.dt.bfloat16
AF = mybir.ActivationFunctionType
ALU = mybir.AluOpType
AX = mybir.AxisListType

D = 1024
DFF = 2816
NFF = DFF // 128
DEPTH = 4
HYB_IN = 3096
EPS = 1e-6
BIG = 30000.0
ENGS = ("pe", "act", "dve", "pool", "sp")
NDMASEM = 24


class Prog:
    def __init__(self, nc):
        self.nc = nc
        self.ops = []
        self.cnt = {e: 0 for e in ENGS}
        self.last_w = {}
        self.readers = {}
        self.ndma = {e: 0 for e in ENGS}
        self.last_on = {e: None for e in ENGS}
        self.recent_dma = {e: [] for e in ENGS}

    def _add(self, eng, fn, reads, writes, dma, extra=()):
        deps = set(extra)
        for b in reads:
            w = self.last_w.get(b)
            if w is not None:
                deps.add(w)
        for b in writes:
            w = self.last_w.get(b)
            if w is not None:
                deps.add(w)
            for r in self.readers.get(b, ()):
                deps.add(r)
        if eng == "pe" and not dma:
            deps = {d for d in deps if not (self.ops[d]["eng"] == "pe" and not self.ops[d]["dma"])}
        oid = len(self.ops)
        op = dict(id=oid, eng=eng, fn=fn, deps=deps, dma=dma, dma_idx=None)
        if dma:
            op["dma_idx"] = self.ndma[eng]
            self.ndma[eng] += 1
            self.recent_dma[eng].append(oid)
            if len(self.recent_dma[eng]) > NDMASEM:
                self.recent_dma[eng].pop(0)
        self.cnt[eng] += 1
        self.ops.append(op)
        self.last_on[eng] = oid
        for b in reads:
            self.readers.setdefault(b, []).append(oid)
        for b in writes:
            self.last_w[b] = oid
            self.readers[b] = []
        return oid

    def op(self, eng, fn, reads=(), writes=()):
        return self._add(eng, fn, tuple(reads), tuple(writes), False)

    def dma(self, eng, fn, reads=(), writes=()):
        return self._add(eng, fn, tuple(reads), tuple(writes), True)

    def barrier(self):
        deps = [o for o in self.last_on.values() if o is not None] + [o for q in self.recent_dma.values() for o in q]
        for e in ENGS:
            self._add(e, lambda eng: eng.nop(), (), (), False, extra=deps)
        self.last_w = {}
        self.readers = {}

    def emit(self, final_wait_eng="sp"):
        nc = self.nc
        ops = self.ops
        signaled = set()
        for op in ops:
            for d in op["deps"]:
                if not ops[d]["dma"]:
                    signaled.add(d)
        sigcount = {}
        run = {e: 0 for e in ENGS}
        for op in ops:
            if op["dma"]:
                continue
            if op["id"] in signaled:
                run[op["eng"]] += 1
                sigcount[op["id"]] = run[op["eng"]]
        by_eng = {e: [o for o in ops if o["eng"] == e] for e in ENGS}
        ndma = self.ndma
        with contextlib.ExitStack() as st:
            esem = {e: st.enter_context(nc.semaphore("s_" + e)) for e in ENGS}
            dqs = [e for e in ENGS if ndma[e] > 0]
            dsem = {q: [st.enter_context(nc.semaphore("d%s_%d" % (q, i))) for i in range(NDMASEM)] for q in dqs}
            block = st.enter_context(nc.Block())
            engobj = {"pe": "tensor", "act": "scalar", "dve": "vector", "pool": "gpsimd", "sp": "sync"}

            def body_for(e):
                def body(eng):
                    waited_e = {x: 0 for x in ENGS}
                    waited_d = {}
                    for op in by_eng[e]:
                        need_e = {}
                        need_d = {}
                        for d in op["deps"]:
                            dop = ops[d]
                            if dop["dma"]:
                                k = dop["dma_idx"]
                                s = (dop["eng"], k % NDMASEM)
                                need_d[s] = max(need_d.get(s, 0), 16 * (k // NDMASEM + 1))
                            else:
                                de = dop["eng"]
                                need_e[de] = max(need_e.get(de, 0), sigcount[d])
                        if op["dma"]:
                            k = op["dma_idx"]
                            if k >= NDMASEM:
                                s = (e, k % NDMASEM)
                                need_d[s] = max(need_d.get(s, 0), 16 * (k // NDMASEM))
                        for de, v in need_e.items():
                            if v > waited_e[de]:
                                eng.wait_ge(esem[de], v)
                                waited_e[de] = v
                        for ds, v in need_d.items():
                            if v > waited_d.get(ds, 0):
                                eng.wait_ge(dsem[ds[0]][ds[1]], v)
                                waited_d[ds] = v
                        ins = op["fn"](eng)
                        if op["dma"]:
                            ins.then_inc(dsem[e][op["dma_idx"] % NDMASEM], 16)
                        elif op["id"] in signaled:
                            ins.then_inc(esem[e], 1)
                    if e == final_wait_eng:
                        for q in dqs:
                            for s in range(NDMASEM):
                                n = (ndma[q] - s + NDMASEM - 1) // NDMASEM if ndma[q] > s else 0
                                if n > 0 and 16 * n > waited_d.get((q, s), 0):
                                    eng.wait_ge(dsem[q][s], 16 * n)
                return body

            for e in ENGS:
                if by_eng[e] or e == final_wait_eng:
                    getattr(block, engobj[e])(body_for(e))


def _layout(T):
    off = {}
    n = 0

    def add(name, w):
        nonlocal n
        off[name] = (n, w)
        n += w
    add("nm", DEPTH * 8)
    add("nf", DEPTH * 8)
    for j in range(2):
        add("gb%d" % j, 2)
        add("gn%d" % j, 4)
        add("cw%d" % j, 16)
        add("bif%d" % j, 1)
        add("mn%d" % j, 4)
        add("qn%d" % j, 1)
        add("kn%d" % j, 1)
    add("bpt", 8 * 34)
    add("pbias", 256)
    add("pmb", 256)
    add("identf", 128)
    add("scr", 8)
    return off, n


def _layout_b():
    off = {}
    n = 0
    for name, w in (("ones", 128), ("ident", 128), ("tri", 128), ("cm", 512), ("E", 2048), ("sel", 512)):
        off[name] = (n, w)
        n += w
    return off, n


def _pack_consts(T, inp):
    off, n = _layout(T)
    cp = np.zeros((128, n), np.float32)

    def put(name, arr):
        o, w = off[name]
        arr = np.asarray(arr, np.float32)
        cp[:arr.shape[0], o:o + arr.shape[1]] = arr

    def fm(v, nch):
        return np.asarray(v, np.float32).reshape(nch, 128).T
    put("nm", np.concatenate([fm(inp["norm_mix"][l], 8) for l in range(DEPTH)], axis=1))
    put("nf", np.concatenate([fm(inp["norm_ffn"][l], 8) for l in range(DEPTH)], axis=1))
    for j in range(2):
        put("gb%d" % j, fm(inp["gla_b_gate"][j], 2))
        put("gn%d" % j, fm(inp["gla_norm"][j], 4))
        cw = np.zeros((128, 16), np.float32)
        conv = np.asarray(inp["ml_conv"][j], np.float32)
        for pp in range(2):
            for qk in range(2):
                for tap in range(4):
                    cw[:, pp * 8 + qk * 4 + tap] = conv[tap, qk * 256 + pp * 128: qk * 256 + pp * 128 + 128]
        put("cw%d" % j, cw)
        put("bif%d" % j, np.asarray(inp["ml_b_if"][j], np.float32).reshape(8, 1))
        put("mn%d" % j, fm(inp["ml_norm"][j], 4))
        put("qn%d" % j, np.asarray(inp["moba_q_norm"][j], np.float32).reshape(128, 1))
        put("kn%d" % j, np.asarray(inp["moba_k_norm"][j], np.float32).reshape(128, 1))
    bpt = np.zeros((128, 8 * 34), np.float32)
    sl = np.arange(128, dtype=np.float32)
    for h in range(8):
        slope = 2.0 ** (-(h + 1))
        for ri in range(34):
            r = ri - 1
            bpt[:, h * 34 + ri] = -slope * (128.0 * r + 255.0 - sl)
    put("bpt", bpt)
    pb = np.zeros((128, 256), np.float32)
    pm = np.zeros((128, 256), np.float32)
    for qb in range(16):
        for nb in range(16):
            pb[:, qb * 16 + nb] = 0.0 if nb < qb else -1e30
            pm[:, qb * 16 + nb] = BIG if nb < qb else 0.0
    put("pbias", pb)
    put("pmb", pm)
    sel = np.zeros((128, 512), np.float32)
    for pp in range(2):
        for hh in range(2):
            sel[4 + 2 * pp + hh, (pp * 2 + 0) * 128 + 64 * hh:(pp * 2 + 0) * 128 + 64 * hh + 64] = 1.0
            sel[2 * pp + hh, (pp * 2 + 1) * 128 + 64 * hh:(pp * 2 + 1) * 128 + 64 * hh + 64] = 1.0
    put("identf", np.eye(128, dtype=np.float32))
    offb, nb_ = _layout_b()
    cb = np.zeros((128, nb_), np.float32)
    o, w = offb["ones"]; cb[:, o:o + w] = 1.0
    o, w = offb["ident"]; cb[:, o:o + w] = np.eye(128)
    o, w = offb["tri"]; cb[:, o:o + w] = np.triu(np.ones((128, 128)))
    o, w = offb["cm"]
    s_ = np.arange(128)[:, None]
    t_ = np.arange(256)[None, :]
    for jj in range(2):
        cb[:, o + jj * 256:o + (jj + 1) * 256] = np.where(t_ >= 128 * jj + s_, 0.0, -BIG)
    o, w = offb["E"]
    for jb in range(16):
        cb[jb, o + jb * 128:o + (jb + 1) * 128] = 1.0
        cb[16, o + jb * 128:o + (jb + 1) * 128] = 1.0
    o, w = offb["sel"]; cb[:, o:o + w] = sel
    al = np.zeros((8, T), np.float32)
    tl = np.arange(T) % 256
    for h in range(8):
        al[h] = (2.0 ** (-(h + 1))) * (255.0 - tl)
    return cp, cb, al


class K:
    def __init__(self, T, layers=(0, 1, 2, 3), dbg=None):
        self.T = T
        self.NT = T // 512
        self.layers = layers
        self.dbg = dbg
        nc = self.nc = bass.Bass("TRN2", target_bir_lowering=False)
        self.p = Prog(nc)
        self.off, self.ncp = _layout(T)
        self.offb, self.ncb = _layout_b()
        dt = nc.dram_tensor
        self.xT_in = dt("xT", [D, T], F32, kind="ExternalInput").ap()
        self.cp_in = dt("cpack", [128, self.ncp], F32, kind="ExternalInput").ap()
        self.cb_in = dt("cbf", [128, self.ncb], F32, kind="ExternalInput").ap()
        self.al_in = dt("alrow", [8, T], F32, kind="ExternalInput").ap()
        self.w_in = dt("hyb_w_in", [2, D, HYB_IN], F32, kind="ExternalInput").ap()
        self.w_g2 = dt("gla_w_gate2", [2, 16, 256], F32, kind="ExternalInput").ap()
        self.w_ho = dt("hyb_w_out", [2, D, D], F32, kind="ExternalInput").ap()
        self.w_qkv = dt("moba_w_qkv", [2, D, 3 * D], F32, kind="ExternalInput").ap()
        self.w_mo = dt("moba_w_o", [2, D, D], F32, kind="ExternalInput").ap()
        self.w_fg = dt("ffn_w_gate", [DEPTH, D, DFF], F32, kind="ExternalInput").ap()
        self.w_fu = dt("ffn_w_up", [DEPTH, D, DFF], F32, kind="ExternalInput").ap()
        self.w_fd = dt("ffn_w_down", [DEPTH, DFF, D], F32, kind="ExternalInput").ap()
        self.outT = dt("outT", [D, T], F32, kind="ExternalOutput").ap()
        self.xres = dt("xres", [D, T], F32, kind="Internal").ap()
        self.yT = dt("yTs", [D, T], BF16, kind="Internal").ap()

    def un(self, name):
        self._uid = getattr(self, "_uid", 0) + 1
        return "%s_%d" % (name, self._uid)

    def mm(self, out, lhsT, rhs, start, stop, r, w):
        self.p.op("pe", lambda e: e.matmul(out, lhsT=lhsT, rhs=rhs, start=start, stop=stop), r, w)

    def tr(self, out, in_, ident, r, w):
        self.p.op("pe", lambda e: e.transpose(out, in_, ident), r, w)

    def act(self, out, in_, func, r, w, bias=None, scale=None):
        kw = {}
        if bias is not None:
            kw["bias"] = bias
        if scale is not None:
            kw["scale"] = scale
        self.p.op("act", lambda e: e.activation(out=out, in_=in_, func=func, **kw), r, w)

    def tt(self, eng, out, in0, in1, op, r, w):
        self.p.op(eng, lambda e: e.tensor_tensor(out=out, in0=in0, in1=in1, op=op), r, w)

    def ts(self, eng, out, in0, s1, s2, op0, op1, r, w):
        if op1 is None:
            self.p.op(eng, lambda e: e.tensor_scalar(out=out, in0=in0, scalar1=s1, scalar2=None, op0=op0), r, w)
        else:
            self.p.op(eng, lambda e: e.tensor_scalar(out=out, in0=in0, scalar1=s1, scalar2=s2, op0=op0, op1=op1), r, w)

    def stt(self, out, in0, scalar, in1, op0, op1, r, w):
        self.p.op("dve", lambda e: e.scalar_tensor_tensor(out=out, in0=in0, scalar=scalar, in1=in1, op0=op0, op1=op1), r, w)

    def cp(self, eng, out, in_, r, w):
        if eng == "act":
            self.p.op("act", lambda e: e.copy(out=out, in_=in_), r, w)
        else:
            self.p.op(eng, lambda e: e.tensor_copy(out=out, in_=in_), r, w)

    def dma(self, eng, out, in_, r, w):
        self.p.dma(eng, lambda e: e.dma_start(out=out, in_=in_), r, w)

    def C(self, name, i=0, n=1):
        o, w = self.off[name]
        return self.CP[:, o + i:o + i + n]

    def CB_(self, name, i=0, n=None):
        o, w = self.offb[name]
        if n is None:
            n = w
        return self.CB[:, o + i:o + i + n]

    def rms_fm(self, xs, gcols, outs, W, nfeat, tag, rx, wx, scale_extra=None):
        nch = len(xs)
        sq = self.sq
        ps = self.ps[6]
        for c in range(nch):
            self.act(sq[:, c % 2, :W], xs[c], AF.Square, rx, ["sq%d" % (c % 2)])
            self.mm(ps[:, :W], self.CB_("ones"), sq[:, c % 2, :W], c == 0, c == nch - 1, ["sq%d" % (c % 2), "CB"], ["ps6"])
        rs = self.rstd
        self.act(rs[:, :W], ps[:, :W], AF.Ln, ["ps6", "CP"], ["rstd"], bias=self.C("scr", 0), scale=1.0 / nfeat)
        self.act(rs[:, :W], rs[:, :W], AF.Exp, ["rstd"], ["rstd"], scale=-0.5)
        for c in range(nch):
            self.stt(outs[c], xs[c], gcols[c], rs[:, :W], ALU.mult, ALU.mult, rx + ["rstd", "CP"], wx[c])

    def setup(self, st):
        nc = self.nc
        sb = lambda name, shape, dt_: st.enter_context(nc.sbuf_tensor(self.un(name), shape, dt_))
        self.CP = sb("CP", [128, self.ncp], F32)
        self.CB = sb("CB", [128, self.ncb], BF16)
        self.ps = [st.enter_context(nc.psum_tensor("ps%d" % i, [128, 512], F32)) for i in range(7)]
        self.psb = st.enter_context(nc.psum_tensor("psb", [128, 1024], BF16))
        self.sq = sb("sq", [128, 2, 512], BF16)
        self.rstd = sb("rstd", [128, 512], F32)
        self.dma("sp", self.CP[:], self.cp_in, [], ["CP"])
        self.dma("pool", self.CB[:], self.cb_in, [], ["CB"])
        o, _ = self.off["scr"]
        self.p.op("dve", lambda e: e.memset(self.CP[:, o:o + 1], EPS), ["CP"], ["CP"])
        self.p.op("dve", lambda e: e.memset(self.CP[:, o + 1:o + 2], 1.0), ["CP"], ["CP"])
        for j in range(2):
            self.ts("dve", self.C("qn%d" % j), self.C("qn%d" % j), float(128 ** -0.5), None, ALU.mult, None, ["CP"], ["CP"])
            self.ts("dve", self.C("gb%d" % j, 0, 2), self.C("gb%d" % j, 0, 2), -1.0, None, ALU.mult, None, ["CP"], ["CP"])

    def phase_c(self, l, w_o_ap, last):
        nc, T = self.nc, self.T
        HF = NFF // 2
        with contextlib.ExitStack() as st:
            sb = lambda name, shape, dt_: st.enter_context(nc.sbuf_tensor(self.un(name), shape, dt_))
            wo = sb("wo", [128, 8, D], BF16)
            wg = sb("wg", [128, 8, DFF], BF16)
            wu = sb("wu", [128, 8, DFF], BF16)
            wd = sb("wd", [128, NFF, D], BF16)
            xt = sb("xt", [128, 8, 512], F32)
            hT = sb("hT", [128, HF, 512], BF16)
            xn2 = sb("xn2", [128, 8, 512], BF16)
            sg = [sb("sg%d" % i, [128, 512], BF16) for i in range(2)]
            self.dma("pool", wo[:], w_o_ap.rearrange("(k p) n -> p k n", p=128), [], ["wo"])
            for (wt, src, nm) in ((wg, self.w_fg[l], "wg"), (wu, self.w_fu[l], "wu")):
                v = src.rearrange("(k p) n -> p k n", p=128)
                for hlf in range(2):
                    self.dma("pool", wt[:, :, hlf * 1408:(hlf + 1) * 1408], v[:, :, hlf * 1408:(hlf + 1) * 1408], [], [nm + str(hlf)])
            wdv = self.w_fd[l].rearrange("(f p) n -> p f n", p=128)
            for hlf in range(2):
                self.dma("pool", wd[:, hlf * HF:(hlf + 1) * HF, :], wdv[:, hlf * HF:(hlf + 1) * HF, :], [], ["wd%d" % hlf])
            xv = self.xres.rearrange("(c p) t -> p c t", p=128)
            yv = self.yT.rearrange("(c p) t -> p c t", p=128)
            ov = (self.outT if last else self.xres).rearrange("(c p) t -> p c t", p=128)
            hnames = ["hT%d" % f for f in range(HF)]
            for tt in range(self.NT):
                t0 = tt * 512
                X = xt
                Y = hT[:, 0:8, :]
                self.dma("sp", X[:], xv[:, :, t0:t0 + 512], [], ["xt0"])
                self.dma("sp", Y, yv[:, :, t0:t0 + 512], [], ["yt0"] + hnames)
                for m in range(8):
                    ps = self.ps[m % 2]
                    for k in range(8):
                        self.mm(ps[:], wo[:, k, m * 128:(m + 1) * 128], Y[:, k, :], k == 0, k == 7,
                                ["wo", "yt0"], ["ps%d" % (m % 2)])
                    self.tt("dve", X[:, m, :], ps[:], X[:, m, :], ALU.add, ["ps%d" % (m % 2), "xt0"], ["xt0"])
                gc = [self.C("nf", l * 8 + c) for c in range(8)]
                self.rms_fm([X[:, c, :] for c in range(8)], gc, [xn2[:, c, :] for c in range(8)], 512, D,
                            "c", ["xt0"], [["xn2"]] * 8)
                for hlf in range(2):
                    for fi in range(HF):
                        f = hlf * HF + fi
                        pg, pu = self.ps[2 + (f % 2)], self.ps[4 + (f % 2)]
                        wh = "0" if f * 128 < 1408 else "1"
                        for k in range(8):
                            self.mm(pg[:], wg[:, k, f * 128:(f + 1) * 128], xn2[:, k, :], k == 0, k == 7,
                                    ["wg" + wh, "xn2"], ["ps%d" % (2 + f % 2)])
                        for k in range(8):
                            self.mm(pu[:], wu[:, k, f * 128:(f + 1) * 128], xn2[:, k, :], k == 0, k == 7,
                                    ["wu" + wh, "xn2"], ["ps%d" % (4 + f % 2)])
                        self.act(sg[f % 2][:], pg[:], AF.Silu, ["ps%d" % (2 + f % 2)], ["sg%d" % (f % 2)])
                        self.tt("dve", hT[:, fi, :], sg[f % 2][:], pu[:], ALU.mult, ["sg%d" % (f % 2), "ps%d" % (4 + f % 2)],
                                ["hT%d" % fi, "yt0"])
                    for m in range(8):
                        ps = self.ps[m % 2]
                        for fi in range(HF):
                            f = hlf * HF + fi
                            self.mm(ps[:], wd[:, f, m * 128:(m + 1) * 128], hT[:, fi, :], fi == 0, fi == HF - 1,
                                    ["wd%d" % hlf, "hT%d" % fi], ["ps%d" % (m % 2)])
                        self.tt("dve", X[:, m, :], ps[:], X[:, m, :], ALU.add, ["ps%d" % (m % 2), "xt0"], ["xt0"])
                self.dma("sp", ov[:, :, t0:t0 + 512], X[:], ["xt0"], [])
        self.p.barrier()

    def phase_a(self, l, xnT, src, st):
        nc = self.nc
        xa = [st.enter_context(nc.sbuf_tensor(self.un("xa%d" % i), [128, 8, 512], F32)) for i in range(2)]
        xv = src.rearrange("(c p) t -> p c t", p=128)
        for tt in range(self.NT):
            s = tt % 2
            t0 = tt * 512
            self.dma("sp", xa[s][:], xv[:, :, t0:t0 + 512], [], ["xa%d" % s])
            gc = [self.C("nm", l * 8 + c) for c in range(8)]
            self.rms_fm([xa[s][:, c, :] for c in range(8)], gc, [xnT[:, c, t0:t0 + 512] for c in range(8)], 512, D,
                        "a", ["xa%d" % s], [["xnT%d" % tt]] * 8)

    def phase_moba(self, l, xnT, st):
        nc, T, NT = self.nc, self.T, self.NT
        j = l // 2
        NB = T // 256
        sb = lambda name, shape, dt_: st.enter_context(nc.sbuf_tensor(self.un(name), shape, dt_))
        wq = [sb("wq%d" % i, [128, 8, 384], BF16) for i in range(2)]
        qT = sb("qT", [128, T], BF16)
        kT = sb("kT", [128, T], BF16)
        V = sb("V", [128, T // 128, 128], BF16)
        kmf = sb("kmf", [128, 16], F32)
        kmb = sb("kmb", [128, 16], BF16)
        SELT = sb("SELT", [17, T], BF16)
        gm = sb("gm", [128, 16], F32)
        top8 = sb("top8", [128, 8], F32)
        selb = sb("selb", [128, 16], F32)
        PT = [sb("PT%d" % i, [128, 256], BF16) for i in range(3)]
        rden = [sb("rden%d" % i, [128, 256], F32) for i in range(2)]
        yh = [sb("yh%d" % i, [128, T], BF16) for i in range(2)]
        wv_ = self.w_qkv[j].rearrange("(k p) n -> p k n", p=128)
        yv = self.yT
        for h in range(8):
            hs = h % 2
            W = wq[hs]
            for part in range(3):
                self.dma("pool", W[:, :, part * 128:(part + 1) * 128], wv_[:, :, part * D + h * 128: part * D + (h + 1) * 128],
                         [], ["wq%d_%d" % (hs, part)])
            self.dma("pool", SELT[16:17, :], self.al_in[h:h + 1, :], [], ["SELTa"])
            for tt in range(NT):
                t0 = tt * 512
                for qi, (dst, gn) in enumerate(((qT, "qn%d" % j), (kT, "kn%d" % j))):
                    ps = self.ps[qi]
                    for k in range(8):
                        self.mm(ps[:], W[:, k, qi * 128:(qi + 1) * 128], xnT[:, k, t0:t0 + 512], k == 0, k == 7,
                                ["wq%d_%d" % (hs, qi), "xnT%d" % tt], ["ps%d" % qi])
                    self.rms_fm([ps[:]], [self.C(gn)], [dst[:, t0:t0 + 512]], 512, 128, "m", ["ps%d" % qi],
                                [["%sT%d" % ("qk"[qi], tt)]])
                for u in range(4):
                    ti = tt * 4 + u
                    ps = self.ps[2 + (ti % 2)]
                    for k in range(8):
                        self.mm(ps[:, :128], xnT[:, k, ti * 128:(ti + 1) * 128], W[:, k, 256:384], k == 0, k == 7,
                                ["wq%d_2" % hs, "xnT%d" % tt], ["ps%d" % (2 + ti % 2)])
                    self.cp("act", V[:, ti, :], ps[:, :128], ["ps%d" % (2 + ti % 2)], ["V%d" % (ti // 2)])
            self.p.op("dve", lambda e: e.tensor_reduce(out=kmf[:, :NB], in_=kT[:].rearrange("p (n s) -> p n s", s=256),
                                                      axis=AX.X, op=ALU.add),
                      ["kT%d" % tt for tt in range(NT)], ["kmf"])
            self.ts("dve", kmb[:, :NB], kmf[:, :NB], 1.0 / 256.0, None, ALU.mult, None, ["kmf"], ["kmb"])
            if NB < 16:
                self.p.op("dve", lambda e: e.memset(gm[:], -1e30), [], ["gm"])
            for qt in range(T // 128):
                qb = qt // 2
                ps = self.ps[2 + (qt % 2)]
                self.mm(ps[:, :NB], qT[:, qt * 128:(qt + 1) * 128], kmb[:, :NB], True, True,
                        ["qT%d" % (qt // 4), "kmb"], ["ps%d" % (2 + qt % 2)])
                self.tt("dve", gm[:, :NB], ps[:, :NB], self.C("pbias", qb * 16, NB), ALU.add, ["ps%d" % (2 + qt % 2), "CP"], ["gm"])
                self.p.op("dve", lambda e: e.max(out=top8[:], in_=gm[:]), ["gm"], ["top8"])
                self.ts("dve", selb[:], gm[:], top8[:, 2:3], 1.0, ALU.is_ge, ALU.subtract, ["gm", "top8"], ["selb"])
                self.tt("dve", selb[:], selb[:], self.C("pmb", qb * 16, 16), ALU.mult, ["selb", "CP"], ["selb"])
                pt = self.ps[4 + (qt % 2)]
                self.tr(pt[:16, :128], selb[:], self.C("identf", 0, 128), ["selb", "CP"], ["ps%d" % (4 + qt % 2)])
                self.cp("act", SELT[0:16, qt * 128:(qt + 1) * 128], pt[:16, :128], ["ps%d" % (4 + qt % 2)], ["SELT%d" % (qt // 2)])
            tiles = [(qb, kt) for qb in range(NB) for kt in range(2 * qb + 2)]
            oE = self.offb["E"][0]

            def issue_scores(i):
                qb, kt = tiles[i]
                t0 = qb * 256
                bank = i % 2
                pss = self.ps[bank]
                diag = kt >= 2 * qb
                self.mm(pss[:, :256], kT[:, kt * 128:(kt + 1) * 128], qT[:, t0:t0 + 256], True, False,
                        ["kT%d" % (kt // 4), "qT%d" % (qb // 2)], ["ps%d" % bank])
                self.mm(pss[:, :256], self.CB[0:17, oE + (kt // 2) * 128:oE + (kt // 2 + 1) * 128], SELT[0:17, t0:t0 + 256],
                        False, not diag, ["CB", "SELT%d" % qb, "SELTa"], ["ps%d" % bank])
                if diag:
                    self.mm(pss[:, :256], self.CB_("ident"), self.CB_("cm", (kt - 2 * qb) * 256, 256), False, True,
                            ["CB"], ["ps%d" % bank])

            def issue_rest(i):
                qb, kt = tiles[i]
                t0 = qb * 256
                bank = i % 2
                pss = self.ps[bank]
                nk = 2 * qb + 2
                ob, db = 2 + 2 * (qb % 2), 3 + 2 * (qb % 2)
                pso, psd = self.ps[ob], self.ps[db]
                P = PT[i % 3]
                r = 2 * qb - kt
                self.act(P[:], pss[:, :256], AF.Exp, ["ps%d" % bank, "CP"], ["PT%d" % (i % 3)],
                         bias=self.C("bpt", h * 34 + r + 1))
                self.mm(pso[:, :256], V[:, kt, :], P[:], kt == 0, kt == nk - 1, ["V%d" % (kt // 2), "PT%d" % (i % 3)], ["ps%d" % ob])
                self.mm(psd[:, :256], self.CB_("ones"), P[:], kt == 0, kt == nk - 1, ["CB", "PT%d" % (i % 3)], ["ps%d" % db])
                if kt == nk - 1:
                    rd = rden[qb % 2]
                    self.p.op("dve", lambda e: e.reciprocal(out=rd[:], in_=psd[:, :256]), ["ps%d" % db], ["rden%d" % (qb % 2)])
                    self.tt("dve", yh[hs][:, t0:t0 + 256], pso[:, :256], rd[:], ALU.mult, ["ps%d" % ob, "rden%d" % (qb % 2)], ["yh%d" % hs])

            issue_scores(0)
            for i in range(len(tiles)):
                if i + 1 < len(tiles):
                    issue_scores(i + 1)
                issue_rest(i)
            self.dma("sp", yv[h * 128:(h + 1) * 128, :], yh[hs][:], ["yh%d" % hs], ["yT"])

    def phase_hybrid(self, l, xnT, st):
        nc, T, NT = self.nc, self.T, self.NT
        j = l // 2
        NCH = T // 128
        sb = lambda name, shape, dt_: st.enter_context(nc.sbuf_tensor(self.un(name), shape, dt_))
        wq = sb("hwq", [128, 8, 128], BF16)
        wk = sb("hwk", [128, 8, 128], BF16)
        wv = sb("hwv", [128, 8, 256], BF16)
        wgt = sb("hwg", [128, 8, 256], BF16)
        wlr = sb("hwlr", [128, 8, 16], BF16)
        wif = sb("hwif", [128, 8, 8], BF16)
        wg2 = sb("hwg2", [16, 256], BF16)
        qT = sb("hqT", [128, T], BF16)
        kT = sb("hkT", [128, T], BF16)
        QR = [sb("hQR%d" % i, [128, 515], F32) for i in range(2)]
        ACC0 = sb("hACC0", [128, 512], F32)
        ACC = [ACC0, ACC0]
        V = sb("hV", [128, NCH, 256], BF16)
        GA = sb("hGA", [128, 2, 512], BF16)
        lrT = sb("hlrT", [16, T], BF16)
        GT8 = sb("hGT8", [8, T], BF16)
        LF8 = lrT[0:8, :]
        LFt = sb("hLFt", [8, 512], F32)
        Bg = sb("hBg", [128, T + 1], F32)
        NBt = sb("hNB", [128, T], F32)
        BC = sb("hBC", [128, NCH + 1], F32)
        NBC = sb("hNBC", [128, NCH + 1], F32)
        onesf = sb("honesf", [128, 512], BF16)
        E1 = [sb("hE1_%d" % i, [128, 128], F32) for i in range(2)]
        E2 = sb("hE2", [128, 128], F32)
        E3 = sb("hE3", [128, 128], F32)
        qc = [sb("hqc%d" % i, [128, 128], BF16) for i in range(2)]
        kc = [sb("hkc%d" % i, [128, 128], BF16) for i in range(2)]
        kh = sb("hkh", [128, 128], BF16)
        khT = [sb("hkhT%d" % i, [128, 128], BF16) for i in range(2)]
        AT = [sb("hAT%d" % i, [128, 128], BF16) for i in range(4)]
        S32 = sb("hS32", [128, 128], F32)
        Sb = sb("hSb", [128, 128], BF16)
        N32 = sb("hN32", [128, 128], F32)
        Nb = sb("hNb", [128, 128], BF16)
        OT = sb("hOT", [128, 2, 512], F32)
        DD = sb("hDD", [128, 2, 128], F32)
        yo = [sb("hyo%d" % i, [128, 2, 512], BF16) for i in range(2)]
        psb = self.psb
        win = self.w_in[j].rearrange("(k p) n -> p k n", p=128)
        yv = self.yT
        osel = self.offb["sel"][0]

        def load_w(dst, c0, n, nm):
            self.dma("pool", dst[:, :, :n], win[:, :, c0:c0 + n], [], [nm])

        self.p.op("dve", lambda e: e.memset(onesf[:], 1.0), [], ["onesf"])
        self.p.op("dve", lambda e: e.memset(Bg[:, 0:1], 0.0), [], ["Bg0"])
        load_w(wlr, 1536, 16, "hwlr")
        load_w(wif, 3088, 8, "hwif")
        self.dma("pool", wg2[:], self.w_g2[j], [], ["hwg2"])
        obif = self.off["bif%d" % j][0]
        for tt in range(NT):
            t0 = tt * 512
            ps = self.ps[0]
            for k in range(8):
                self.mm(ps[:16, :], wlr[:, k, :], xnT[:, k, t0:t0 + 512], k == 0, k == 7, ["hwlr", "xnT%d" % tt], ["ps0"])
            self.cp("act", lrT[:, t0:t0 + 512], ps[:16, :], ["ps0"], ["hlrT%d" % tt])

        def gate_rows():
            for tt in range(NT):
                t0 = tt * 512
                ps = self.ps[1]
                for k in range(8):
                    self.mm(ps[:8, :], wif[:, k, :], xnT[:, k, t0:t0 + 512], k == 0, k == 7, ["hwif", "xnT%d" % tt], ["ps1"])
                self.act(GT8[:, t0:t0 + 512], ps[:8, :], AF.Identity, ["ps1", "CP"], ["hGT8"], bias=self.CP[0:8, obif:obif + 1])
                self.act(LFt[:], ps[:8, :], AF.Identity, ["ps1", "CP"], ["hLFt"], bias=self.CP[0:8, obif:obif + 1])
                self.act(LFt[:], LFt[:], AF.Exp, ["hLFt"], ["hLFt"], scale=-1.0)
                self.act(LFt[:], LFt[:], AF.Ln, ["hLFt", "CP"], ["hLFt"], bias=self.CP[0:8, self.off["scr"][0] + 1:self.off["scr"][0] + 2])
                self.ts("dve", LF8[:, t0:t0 + 512], LFt[:], -1.0, None, ALU.mult, None, ["hLFt"], ["hLF8"])

        for pair in range(4):
            ml = pair >= 2
            pp = pair % 2
            if pair == 2:
                gate_rows()
            if not ml:
                cq, ck, cv, cg = 128 * pp, 256 + 128 * pp, 512 + 256 * pp, 1024 + 256 * pp
            else:
                cq, ck, cv, cg = 1552 + 128 * pp, 1808 + 128 * pp, 2064 + 256 * pp, 2576 + 256 * pp
            load_w(wq, cq, 128, "hwq")
            load_w(wk, ck, 128, "hwk")
            load_w(wv, cv, 256, "hwv")
            load_w(wgt, cg, 256, "hwg")
            gnm = ("mn%d" if ml else "gn%d") % j
            for qi, (wt, dst, nm) in enumerate(((wq, qT, "hwq"), (wk, kT, "hwk"))):
                dn = "h%sT" % "qk"[qi]
                for tt in range(NT):
                    t0 = tt * 512
                    s = tt % 2
                    ps = self.ps[tt % 2]
                    for k in range(8):
                        self.mm(ps[:], wt[:, k, :], xnT[:, k, t0:t0 + 512], k == 0, k == 7, [nm, "xnT%d" % tt], ["ps%d" % (tt % 2)])
                    if not ml:
                        self.cp("act", dst[:, t0:t0 + 512], ps[:], ["ps%d" % (tt % 2)], [dn])
                    else:
                        self.cp("act", QR[s][:, 3:515], ps[:], ["ps%d" % (tt % 2)], ["hQR%d" % s])
                        if tt == 0:
                            self.p.op("dve", lambda e, s=s: e.memset(QR[s][:, 0:3], 0.0), [], ["hQRh%d" % s])
                        else:
                            self.cp("dve", QR[s][:, 0:3], QR[1 - s][:, 512:515], ["hQR%d" % (1 - s)], ["hQRh%d" % s])
                        o = self.off["cw%d" % j][0] + pp * 8 + qi * 4
                        rd = ["hQR%d" % s, "hQRh%d" % s, "CP"]
                        self.ts("dve", ACC[s][:], QR[s][:, 0:512], self.CP[:, o:o + 1], None, ALU.mult, None, rd, ["hACC0"])
                        for tap in range(1, 4):
                            self.stt(ACC[s][:], QR[s][:, tap:tap + 512], self.CP[:, o + tap:o + tap + 1], ACC[s][:], ALU.mult, ALU.add,
                                     rd + ["hACC0"], ["hACC0"])
                        self.act(dst[:, t0:t0 + 512], ACC[s][:], AF.Silu, ["hACC0"], [dn])
            for tt in range(NT):
                t0 = tt * 512
                for u in range(4):
                    ti = tt * 4 + u
                    ps = self.ps[4 + (ti % 2)]
                    for k in range(8):
                        self.mm(ps[:, :256], xnT[:, k, ti * 128:(ti + 1) * 128], wv[:, k, :], k == 0, k == 7,
                                ["hwv", "xnT%d" % tt], ["ps%d" % (4 + ti % 2)])
                    self.cp("act", V[:, ti, :], ps[:, :256], ["ps%d" % (4 + ti % 2)], ["hV"])
            for tt in range(NT):
                t0 = tt * 512
                ps = self.ps[2]
                if not ml:
                    self.mm(ps[:], wg2[:, pp * 128:(pp + 1) * 128], lrT[:, t0:t0 + 512], True, True, ["hwg2", "hlrT%d" % tt], ["ps2"])
                    self.act(NBt[:, t0:t0 + 512], ps[:], AF.Exp, ["ps2", "CP"], ["hNB"], bias=self.C("gb%d" % j, pp), scale=-1.0)
                    self.act(NBt[:, t0:t0 + 512], NBt[:, t0:t0 + 512], AF.Ln, ["hNB", "CP"], ["hNB"], bias=self.C("scr", 1))
                    self.ts("dve", NBt[:, t0:t0 + 512], NBt[:, t0:t0 + 512], -1.0 / 16.0, None, ALU.mult, None, ["hNB"], ["hNB"])
                else:
                    o = osel + (pp * 2 + 0) * 128
                    self.mm(ps[:], self.CB[0:8, o:o + 128], LF8[:, t0:t0 + 512], True, True, ["CB", "hLF8"], ["ps2"])
                    self.cp("act", NBt[:, t0:t0 + 512], ps[:], ["ps2"], ["hNB"])
                ini = Bg[:, t0:t0 + 1]
                self.p.op("dve", lambda e, t0=t0, ini=ini: e.tensor_tensor_scan(out=Bg[:, 1 + t0:1 + t0 + 512], data0=onesf[:],
                                                                                   data1=NBt[:, t0:t0 + 512], initial=ini,
                                                                                   op0=ALU.mult, op1=ALU.add),
                          ["hNB", "onesf", "hBg", "Bg0"], ["hBg"])
            self.cp("dve", BC[:, 0:NCH], Bg[:, 0:T].rearrange("p (n s) -> p n s", s=128)[:, :, 0], ["hBg", "Bg0"], ["hBC"])
            self.cp("dve", BC[:, NCH:NCH + 1], Bg[:, T:T + 1], ["hBg", "Bg0"], ["hBC"])
            self.ts("dve", NBC[:, :], BC[:, :], -1.0, None, ALU.mult, None, ["hBC"], ["hNBC"])
            for tt in range(NT):
                t0 = tt * 512
                if not ml:
                    self.ts("dve", NBt[:, t0:t0 + 512], Bg[:, 1 + t0:1 + t0 + 512], -1.0, None, ALU.mult, None, ["hBg"], ["hNB"])
                else:
                    ps = self.ps[2]
                    o = osel + (pp * 2 + 1) * 128
                    self.mm(ps[:], self.CB[0:8, o:o + 128], GT8[:, t0:t0 + 512], True, True, ["CB", "hGT8"], ["ps2"])
                    self.tt("dve", NBt[:, t0:t0 + 512], ps[:], Bg[:, 1 + t0:1 + t0 + 512], ALU.subtract, ["ps2", "hBg"], ["hNB"])
            self.p.barrier()
            self.p.op("dve", lambda e: e.memset(S32[:], 0.0), [], ["hS32"])
            self.p.op("dve", lambda e: e.memset(Sb[:], 0.0), [], ["hSb"])
            if ml:
                self.p.op("dve", lambda e: e.memset(N32[:], 0.0), [], ["hN32"])
                self.p.op("dve", lambda e: e.memset(Nb[:], 0.0), [], ["hNb"])
            def stage1(ci):
                c0 = ci * 128
                s2 = ci % 2
                sl = slice(c0, c0 + 128)
                e1 = E1[s2]
                self.act(e1[:], Bg[:, 1 + c0:1 + c0 + 128], AF.Exp, ["hBg", "hNBC"], ["hE1_%d" % s2], bias=NBC[:, ci:ci + 1])
                self.act(E2[:], NBt[:, sl], AF.Exp, ["hNB", "hBC"], ["hE2"], bias=BC[:, ci:ci + 1])
                self.act(E3[:], NBt[:, sl], AF.Exp, ["hNB", "hBC"], ["hE3"], bias=BC[:, ci + 1:ci + 2])
                self.stt(qc[s2][:], qT[:, sl], 0.125, e1[:], ALU.mult, ALU.mult, ["hqT", "hE1_%d" % s2], ["hqc%d" % s2])
                self.tt("pool", kc[s2][:], kT[:, sl], E2[:], ALU.mult, ["hkT", "hE2"], ["hkc%d" % s2])
                self.tt("pool", kh[:], kT[:, sl], E3[:], ALU.mult, ["hkT", "hE3"], ["hkh"])
                self.tr(psb[:, :128], kh[:], self.CB_("ident"), ["hkh", "CB"], ["psb"])
                self.cp("act", khT[s2][:], psb[:, :128], ["psb"], ["hkhT%d" % s2])
                for hh in range(2):
                    hp = slice(64 * hh, 64 * hh + 64)
                    self.mm(self.ps[hh][:, 0:128], kc[s2][hp, :], qc[s2][hp, :], True, True, ["hkc%d" % s2, "hqc%d" % s2], ["ps%d" % hh])
                for hh in range(2):
                    self.tt("dve", AT[s2 * 2 + hh][:], self.ps[hh][:, 0:128], self.CB_("tri"), ALU.mult, ["ps%d" % hh, "CB"], ["hAT%d" % (s2 * 2 + hh)])

            stage1(0)
            for ci in range(NCH):
                c0 = ci * 128
                s2 = ci % 2
                u = ci % 4
                sl = slice(c0, c0 + 128)
                e1 = E1[s2]
                if ci + 1 < NCH:
                    stage1(ci + 1)
                psU = self.ps[5]
                for hh in range(2):
                    hp = slice(64 * hh, 64 * hh + 64)
                    psO, psD = self.ps[2 + hh], self.ps[4]
                    nO, nD = "ps%d" % (2 + hh), "ps4"
                    at = AT[s2 * 2 + hh]
                    an = "hAT%d" % (s2 * 2 + hh)
                    self.mm(psO[:, 0:128], V[:, ci, hh * 128:(hh + 1) * 128], at[:], True, False, ["hV", an], [nO])
                    self.mm(psO[:, 0:128], Sb[hp, :], qc[s2][hp, :], False, True, ["hSb", "hqc%d" % s2], [nO])
                    if ml:
                        self.mm(psD[:, hh * 128:(hh + 1) * 128], self.CB_("ones"), at[:], True, False, ["CB", an], [nD])
                        self.mm(psD[:, hh * 128:(hh + 1) * 128], Nb[hp, :], qc[s2][hp, :], False, True, ["hNb", "hqc%d" % s2], [nD])
                    self.mm(psU[hp, 0:128], khT[s2][:, hp], V[:, ci, hh * 128:(hh + 1) * 128], True, True, ["hkhT%d" % s2, "hV"], ["ps5"])
                    if ml:
                        self.mm(psU[hp, 128:256], khT[s2][:, hp], self.CB_("ones"), True, True, ["hkhT%d" % s2, "CB"], ["ps5"])
                    if not ml:
                        self.cp("act", OT[:, hh, u * 128:(u + 1) * 128], psO[:, 0:128], [nO], ["hOT"])
                    else:
                        self.act(DD[:, hh, :], psD[:, hh * 128:(hh + 1) * 128], AF.Abs, [nD], ["hDD%d" % hh])
                        self.ts("dve", DD[:, hh, :], DD[:, hh, :], 1.0, None, ALU.max, None, ["hDD%d" % hh], ["hDD%d" % hh])
                        self.p.op("dve", lambda e, hh=hh: e.reciprocal(out=DD[:, hh, :], in_=DD[:, hh, :]), ["hDD%d" % hh], ["hDD%d" % hh])
                        self.tt("dve", OT[:, hh, u * 128:(u + 1) * 128], psO[:, 0:128], DD[:, hh, :], ALU.mult, [nO, "hDD%d" % hh], ["hOT"])
                self.stt(S32[:], S32[:], e1[:, 127:128], psU[:, 0:128], ALU.mult, ALU.add, ["hS32", "hE1_%d" % s2, "ps5"], ["hS32"])
                self.cp("act", Sb[:], S32[:], ["hS32"], ["hSb"])
                if ml:
                    self.stt(N32[:], N32[:], e1[:, 127:128], psU[:, 128:256], ALU.mult, ALU.add, ["hN32", "hE1_%d" % s2, "ps5"], ["hN32"])
                    self.cp("act", Nb[:], N32[:], ["hN32"], ["hNb"])
                if u == 3:
                    tt = ci // 4
                    t0 = tt * 512
                    Y = yo[tt % 2]
                    for hh in range(2):
                        head = 2 * pp + hh
                        ps = self.ps[hh]
                        for k in range(8):
                            self.mm(ps[:], wgt[:, k, hh * 128:(hh + 1) * 128], xnT[:, k, t0:t0 + 512], k == 0, k == 7,
                                    ["hwg", "xnT%d" % tt], ["ps%d" % hh])
                        self.act(GA[:, hh, :], ps[:], AF.Sigmoid if ml else AF.Silu, ["ps%d" % hh], ["hGA%d" % hh])
                        if ml:
                            self.tt("pool", OT[:, hh, :], OT[:, hh, :], GA[:, hh, :], ALU.mult, ["hOT", "hGA%d" % hh], ["hOT"])
                            self.rms_fm([OT[:, hh, :]], [self.C(gnm, head)], [Y[:, hh, :]], 512, 128, "h", ["hOT"], [["hyo%d" % (tt % 2)]])
                        else:
                            self.rms_fm([OT[:, hh, :]], [self.C(gnm, head)], [OT[:, hh, :]], 512, 128, "h", ["hOT"], [["hOT"]])
                            self.tt("pool", Y[:, hh, :], OT[:, hh, :], GA[:, hh, :], ALU.mult, ["hOT", "hGA%d" % hh], ["hyo%d" % (tt % 2)])
                    r0 = (512 if ml else 0) + pp * 256
                    self.dma("sp", yv[r0:r0 + 256, t0:t0 + 512].rearrange("(h p) t -> p h t", p=128), Y[:], ["hyo%d" % (tt % 2)], ["yT"])
            self.p.barrier()


    def build(self):
        nc = self.nc
        with contextlib.ExitStack() as st:
            self.setup(st)
            first = True
            for li, l in enumerate(self.layers):
                src = self.xT_in if first else self.xres
                with contextlib.ExitStack() as st2:
                    xnT = st2.enter_context(nc.sbuf_tensor(self.un("xnT"), [128, 8, self.T], BF16))
                    with contextlib.ExitStack() as st3:
                        self.phase_a(l, xnT, src, st3)
                    self.p.barrier()
                    with contextlib.ExitStack() as st3:
                        if l % 2 == 0:
                            self.phase_hybrid(l, xnT, st3)
                        else:
                            self.phase_moba(l, xnT, st3)
                    self.p.barrier()
                if first and self.dbg != "y":
                    self.dma("sp", self.xres, self.xT_in, [], ["xres_all"])
                    self.p.barrier()
                first = False
                w_o = (self.w_ho if l % 2 == 0 else self.w_mo)[l // 2]
                if self.dbg == "y":
                    self.dma("pool", self.outT, self.yT, [], [])
                    break
                self.phase_c(l, w_o, last=(li == len(self.layers) - 1))
            self.p.emit()
        return nc


_CACHE = {}


def _get_nc(T):
    if T not in _CACHE:
        _CACHE[T] = K(T).build()
    return _CACHE[T]


def make_in_maps(inputs, T, ncores=8):
    cp, cb, al = _pack_consts(T, inputs)
    x = np.asarray(inputs["x"], np.float32)
    nb = x.shape[0]
    shared = {k: np.ascontiguousarray(np.asarray(inputs[k], np.float32)) for k in
              ("hyb_w_in", "gla_w_gate2", "hyb_w_out", "moba_w_qkv", "moba_w_o", "ffn_w_gate", "ffn_w_up", "ffn_w_down")}
    maps = []
    for c in range(ncores):
        b = c % nb
        m = dict(shared)
        m["xT"] = np.ascontiguousarray(x[b].T)
        m["cpack"] = cp
        m["cbf"] = cb
        m["alrow"] = al
        maps.append(m)
    return maps


def kernel(**inputs):
    x = np.asarray(inputs["x"])
    B, T, _ = x.shape
    nc = _get_nc(T)
    maps = make_in_maps(inputs, T)
    res = run_bass_kernel_spmd(nc, maps, core_ids=list(range(8)))
    out = np.stack([np.ascontiguousarray(res.results[b]["outT"].T) for b in range(B)], axis=0)
    return out.astype(np.float32)
```

```python
import contextlib
import numpy as np
import concourse.bass as bass
import concourse.mybir as mybir
from concourse.bass_utils import run_bass_kernel_spmd

F32 = mybir.dt.float32
BF16 = mybir.dt.bfloat16
AF = mybir.ActivationFunctionType
ALU = mybir.AluOpType
AX = mybir.AxisListType

D = 1024
DFF = 2816
NFF = DFF // 128
DEPTH = 4
HYB_IN = 3096
EPS = 1e-6
BIG = 30000.0
ENGS = ("pe", "act", "dve", "pool", "sp")
NDMASEM = 24


class Prog:
    def __init__(self, nc):
        self.nc = nc
        self.ops = []
        self.cnt = {e: 0 for e in ENGS}
        self.last_w = {}
        self.readers = {}
        self.ndma = {e: 0 for e in ENGS}
        self.last_on = {e: None for e in ENGS}
        self.recent_dma = {e: [] for e in ENGS}

    def _add(self, eng, fn, reads, writes, dma, extra=()):
        deps = set(extra)
        for b in reads:
            w = self.last_w.get(b)
            if w is not None:
                deps.add(w)
        for b in writes:
            w = self.last_w.get(b)
            if w is not None:
                deps.add(w)
            for r in self.readers.get(b, ()):
                deps.add(r)
        if eng == "pe" and not dma:
            deps = {d for d in deps if not (self.ops[d]["eng"] == "pe" and not self.ops[d]["dma"])}
        oid = len(self.ops)
        op = dict(id=oid, eng=eng, fn=fn, deps=deps, dma=dma, dma_idx=None)
        if dma:
            op["dma_idx"] = self.ndma[eng]
            self.ndma[eng] += 1
            self.recent_dma[eng].append(oid)
            if len(self.recent_dma[eng]) > NDMASEM:
                self.recent_dma[eng].pop(0)
        self.cnt[eng] += 1
        self.ops.append(op)
        self.last_on[eng] = oid
        for b in reads:
            self.readers.setdefault(b, []).append(oid)
        for b in writes:
            self.last_w[b] = oid
            self.readers[b] = []
        return oid

    def op(self, eng, fn, reads=(), writes=()):
        return self._add(eng, fn, tuple(reads), tuple(writes), False)

    def dma(self, eng, fn, reads=(), writes=()):
        return self._add(eng, fn, tuple(reads), tuple(writes), True)

    def barrier(self):
        deps = [o for o in self.last_on.values() if o is not None] + [o for q in self.recent_dma.values() for o in q]
        for e in ENGS:
            self._add(e, lambda eng: eng.nop(), (), (), False, extra=deps)
        self.last_w = {}
        self.readers = {}

    def emit(self, final_wait_eng="sp"):
        nc = self.nc
        ops = self.ops
        signaled = set()
        for op in ops:
            for d in op["deps"]:
                if not ops[d]["dma"]:
                    signaled.add(d)
        sigcount = {}
        run = {e: 0 for e in ENGS}
        for op in ops:
            if op["dma"]:
                continue
            if op["id"] in signaled:
                run[op["eng"]] += 1
                sigcount[op["id"]] = run[op["eng"]]
        by_eng = {e: [o for o in ops if o["eng"] == e] for e in ENGS}
        ndma = self.ndma
        with contextlib.ExitStack() as st:
            esem = {e: st.enter_context(nc.semaphore("s_" + e)) for e in ENGS}
            dqs = [e for e in ENGS if ndma[e] > 0]
            dsem = {q: [st.enter_context(nc.semaphore("d%s_%d" % (q, i))) for i in range(NDMASEM)] for q in dqs}
            block = st.enter_context(nc.Block())
            engobj = {"pe": "tensor", "act": "scalar", "dve": "vector", "pool": "gpsimd", "sp": "sync"}

            def body_for(e):
                def body(eng):
                    waited_e = {x: 0 for x in ENGS}
                    waited_d = {}
                    for op in by_eng[e]:
                        need_e = {}
                        need_d = {}
                        for d in op["deps"]:
                            dop = ops[d]
                            if dop["dma"]:
                                k = dop["dma_idx"]
                                s = (dop["eng"], k % NDMASEM)
                                need_d[s] = max(need_d.get(s, 0), 16 * (k // NDMASEM + 1))
                            else:
                                de = dop["eng"]
                                need_e[de] = max(need_e.get(de, 0), sigcount[d])
                        if op["dma"]:
                            k = op["dma_idx"]
                            if k >= NDMASEM:
                                s = (e, k % NDMASEM)
                                need_d[s] = max(need_d.get(s, 0), 16 * (k // NDMASEM))
                        for de, v in need_e.items():
                            if v > waited_e[de]:
                                eng.wait_ge(esem[de], v)
                                waited_e[de] = v
                        for ds, v in need_d.items():
                            if v > waited_d.get(ds, 0):
                                eng.wait_ge(dsem[ds[0]][ds[1]], v)
                                waited_d[ds] = v
                        ins = op["fn"](eng)
                        if op["dma"]:
                            ins.then_inc(dsem[e][op["dma_idx"] % NDMASEM], 16)
                        elif op["id"] in signaled:
                            ins.then_inc(esem[e], 1)
                    if e == final_wait_eng:
                        for q in dqs:
                            for s in range(NDMASEM):
                                n = (ndma[q] - s + NDMASEM - 1) // NDMASEM if ndma[q] > s else 0
                                if n > 0 and 16 * n > waited_d.get((q, s), 0):
                                    eng.wait_ge(dsem[q][s], 16 * n)
                return body

            for e in ENGS:
                if by_eng[e] or e == final_wait_eng:
                    getattr(block, engobj[e])(body_for(e))


def _layout(T):
    off = {}
    n = 0

    def add(name, w):
        nonlocal n
        off[name] = (n, w)
        n += w
    add("nm", DEPTH * 8)
    add("nf", DEPTH * 8)
    for j in range(2):
        add("gb%d" % j, 2)
        add("gn%d" % j, 4)
        add("cw%d" % j, 16)
        add("bif%d" % j, 1)
        add("mn%d" % j, 4)
        add("qn%d" % j, 1)
        add("kn%d" % j, 1)
    add("bpt", 8 * 34)
    add("pbias", 256)
    add("pmb", 256)
    add("identf", 128)
    add("scr", 8)
    return off, n


def _layout_b():
    off = {}
    n = 0
    for name, w in (("ones", 128), ("ident", 128), ("tri", 128), ("cm", 2048), ("E", 2048), ("sel", 512)):
        off[name] = (n, w)
        n += w
    return off, n


def _pack_consts(T, inp):
    off, n = _layout(T)
    cp = np.zeros((128, n), np.float32)

    def put(name, arr):
        o, w = off[name]
        arr = np.asarray(arr, np.float32)
        cp[:arr.shape[0], o:o + arr.shape[1]] = arr

    def fm(v, nch):
        return np.asarray(v, np.float32).reshape(nch, 128).T
    put("nm", np.concatenate([fm(inp["norm_mix"][l], 8) for l in range(DEPTH)], axis=1))
    put("nf", np.concatenate([fm(inp["norm_ffn"][l], 8) for l in range(DEPTH)], axis=1))
    for j in range(2):
        put("gb%d" % j, fm(inp["gla_b_gate"][j], 2))
        put("gn%d" % j, fm(inp["gla_norm"][j], 4))
        cw = np.zeros((128, 16), np.float32)
        conv = np.asarray(inp["ml_conv"][j], np.float32)
        for pp in range(2):
            for qk in range(2):
                for tap in range(4):
                    cw[:, pp * 8 + qk * 4 + tap] = conv[tap, qk * 256 + pp * 128: qk * 256 + pp * 128 + 128]
        put("cw%d" % j, cw)
        put("bif%d" % j, np.asarray(inp["ml_b_if"][j], np.float32).reshape(8, 1))
        put("mn%d" % j, fm(inp["ml_norm"][j], 4))
        put("qn%d" % j, np.asarray(inp["moba_q_norm"][j], np.float32).reshape(128, 1))
        put("kn%d" % j, np.asarray(inp["moba_k_norm"][j], np.float32).reshape(128, 1))
    bpt = np.zeros((128, 8 * 34), np.float32)
    sl = np.arange(128, dtype=np.float32)
    for h in range(8):
        slope = 2.0 ** (-(h + 1))
        for ri in range(34):
            r = ri - 3
            bpt[:, h * 34 + ri] = -slope * (128.0 * r + 511.0 - sl)
    put("bpt", bpt)
    pb = np.zeros((128, 256), np.float32)
    pm = np.zeros((128, 256), np.float32)
    for qb in range(16):
        for nb in range(16):
            pb[:, qb * 16 + nb] = 0.0 if nb < qb else -1e30
            pm[:, qb * 16 + nb] = BIG if nb < qb else 0.0
    put("pbias", pb)
    put("pmb", pm)
    sel = np.zeros((128, 512), np.float32)
    for pp in range(2):
        for hh in range(2):
            sel[4 + 2 * pp + hh, (pp * 2 + 0) * 128 + 64 * hh:(pp * 2 + 0) * 128 + 64 * hh + 64] = 1.0
            sel[2 * pp + hh, (pp * 2 + 1) * 128 + 64 * hh:(pp * 2 + 1) * 128 + 64 * hh + 64] = 1.0
    put("identf", np.eye(128, dtype=np.float32))
    offb, nb_ = _layout_b()
    cb = np.zeros((128, nb_), np.float32)
    o, w = offb["ones"]; cb[:, o:o + w] = 1.0
    o, w = offb["ident"]; cb[:, o:o + w] = np.eye(128)
    o, w = offb["tri"]; cb[:, o:o + w] = np.triu(np.ones((128, 128)))
    o, w = offb["cm"]
    s_ = np.arange(128)[:, None]
    t_ = np.arange(512)[None, :]
    for jj in range(4):
        cb[:, o + jj * 512:o + (jj + 1) * 512] = np.where(t_ >= 128 * jj + s_, 0.0, -BIG)
    o, w = offb["E"]
    for jb in range(16):
        cb[jb, o + jb * 128:o + (jb + 1) * 128] = 1.0
        cb[16, o + jb * 128:o + (jb + 1) * 128] = 1.0
        cb[17, o + jb * 128:o + (jb + 1) * 128] = 1.0
    o, w = offb["sel"]; cb[:, o:o + w] = sel
    al = np.zeros((8, 2, T), np.float32)
    tl = np.arange(T) % 512
    for h in range(8):
        al[h, 0] = (2.0 ** (-(h + 1))) * 256.0 * (1 - tl // 256)
        al[h, 1] = (2.0 ** (-(h + 1))) * (255.0 - tl % 256)
    return cp, cb, al


class K:
    def __init__(self, T, layers=(0, 1, 2, 3), dbg=None):
        self.T = T
        self.NT = T // 512
        self.layers = layers
        self.dbg = dbg
        nc = self.nc = bass.Bass("TRN2", target_bir_lowering=False)
        self.p = Prog(nc)
        self.off, self.ncp = _layout(T)
        self.offb, self.ncb = _layout_b()
        dt = nc.dram_tensor
        self.xT_in = dt("xT", [D, T], F32, kind="ExternalInput").ap()
        self.cp_in = dt("cpack", [128, self.ncp], F32, kind="ExternalInput").ap()
        self.cb_in = dt("cbf", [128, self.ncb], F32, kind="ExternalInput").ap()
        self.al_in = dt("alrow", [8, 2, T], F32, kind="ExternalInput").ap()
        self.w_in = dt("hyb_w_in", [2, D, HYB_IN], F32, kind="ExternalInput").ap()
        self.w_g2 = dt("gla_w_gate2", [2, 16, 256], F32, kind="ExternalInput").ap()
        self.w_ho = dt("hyb_w_out", [2, D, D], F32, kind="ExternalInput").ap()
        self.w_qkv = dt("moba_w_qkv", [2, D, 3 * D], F32, kind="ExternalInput").ap()
        self.w_mo = dt("moba_w_o", [2, D, D], F32, kind="ExternalInput").ap()
        self.w_fg = dt("ffn_w_gate", [DEPTH, D, DFF], F32, kind="ExternalInput").ap()
        self.w_fu = dt("ffn_w_up", [DEPTH, D, DFF], F32, kind="ExternalInput").ap()
        self.w_fd = dt("ffn_w_down", [DEPTH, DFF, D], F32, kind="ExternalInput").ap()
        self.outT = dt("outT", [D, T], F32, kind="ExternalOutput").ap()
        self.xres = dt("xres", [D, T], F32, kind="Internal").ap()
        self.yT = dt("yTs", [D, T], BF16, kind="Internal").ap()

    def un(self, name):
        self._uid = getattr(self, "_uid", 0) + 1
        return "%s_%d" % (name, self._uid)

    def mm(self, out, lhsT, rhs, start, stop, r, w):
        self.p.op("pe", lambda e: e.matmul(out, lhsT=lhsT, rhs=rhs, start=start, stop=stop), r, w)

    def tr(self, out, in_, ident, r, w):
        self.p.op("pe", lambda e: e.transpose(out, in_, ident), r, w)

    def act(self, out, in_, func, r, w, bias=None, scale=None):
        kw = {}
        if bias is not None:
            kw["bias"] = bias
        if scale is not None:
            kw["scale"] = scale
        self.p.op("act", lambda e: e.activation(out=out, in_=in_, func=func, **kw), r, w)

    def tt(self, eng, out, in0, in1, op, r, w):
        self.p.op(eng, lambda e: e.tensor_tensor(out=out, in0=in0, in1=in1, op=op), r, w)

    def ts(self, eng, out, in0, s1, s2, op0, op1, r, w):
        if op1 is None:
            self.p.op(eng, lambda e: e.tensor_scalar(out=out, in0=in0, scalar1=s1, scalar2=None, op0=op0), r, w)
        else:
            self.p.op(eng, lambda e: e.tensor_scalar(out=out, in0=in0, scalar1=s1, scalar2=s2, op0=op0, op1=op1), r, w)

    def stt(self, out, in0, scalar, in1, op0, op1, r, w):
        self.p.op("dve", lambda e: e.scalar_tensor_tensor(out=out, in0=in0, scalar=scalar, in1=in1, op0=op0, op1=op1), r, w)

    def cp(self, eng, out, in_, r, w):
        if eng == "act":
            self.p.op("act", lambda e: e.copy(out=out, in_=in_), r, w)
        else:
            self.p.op(eng, lambda e: e.tensor_copy(out=out, in_=in_), r, w)

    def dma(self, eng, out, in_, r, w):
        self.p.dma(eng, lambda e: e.dma_start(out=out, in_=in_), r, w)

    def C(self, name, i=0, n=1):
        o, w = self.off[name]
        return self.CP[:, o + i:o + i + n]

    def CB_(self, name, i=0, n=None):
        o, w = self.offb[name]
        if n is None:
            n = w
        return self.CB[:, o + i:o + i + n]

    def rms_fm(self, xs, gcols, outs, W, nfeat, tag, rx, wx, scale_extra=None):
        nch = len(xs)
        sq = self.sq
        ps = self.ps[6]
        for c in range(nch):
            self.act(sq[:, c % 2, :W], xs[c], AF.Square, rx, ["sq%d" % (c % 2)])
            self.mm(ps[:, :W], self.CB_("ones"), sq[:, c % 2, :W], c == 0, c == nch - 1, ["sq%d" % (c % 2), "CB"], ["ps6"])
        rs = self.rstd
        self.act(rs[:, :W], ps[:, :W], AF.Ln, ["ps6", "CP"], ["rstd"], bias=self.C("scr", 0), scale=1.0 / nfeat)
        self.act(rs[:, :W], rs[:, :W], AF.Exp, ["rstd"], ["rstd"], scale=-0.5)
        for c in range(nch):
            self.stt(outs[c], xs[c], gcols[c], rs[:, :W], ALU.mult, ALU.mult, rx + ["rstd", "CP"], wx[c])

    def setup(self, st):
        nc = self.nc
        sb = lambda name, shape, dt_: st.enter_context(nc.sbuf_tensor(self.un(name), shape, dt_))
        self.CP = sb("CP", [128, self.ncp], F32)
        self.CB = sb("CB", [128, self.ncb], BF16)
        self.ps = [st.enter_context(nc.psum_tensor("ps%d" % i, [128, 512], F32)) for i in range(7)]
        self.psb = st.enter_context(nc.psum_tensor("psb", [128, 1024], BF16))
        self.sq = sb("sq", [128, 2, 512], BF16)
        self.rstd = sb("rstd", [128, 512], F32)
        self.dma("sp", self.CP[:], self.cp_in, [], ["CP"])
        self.dma("pool", self.CB[:], self.cb_in, [], ["CB"])
        o, _ = self.off["scr"]
        self.p.op("dve", lambda e: e.memset(self.CP[:, o:o + 1], EPS), ["CP"], ["CP"])
        self.p.op("dve", lambda e: e.memset(self.CP[:, o + 1:o + 2], 1.0), ["CP"], ["CP"])
        for j in range(2):
            self.ts("dve", self.C("qn%d" % j), self.C("qn%d" % j), float(128 ** -0.5), None, ALU.mult, None, ["CP"], ["CP"])
            self.ts("dve", self.C("gb%d" % j, 0, 2), self.C("gb%d" % j, 0, 2), -1.0, None, ALU.mult, None, ["CP"], ["CP"])

    def phase_c(self, l, w_o_ap, last):
        nc, T = self.nc, self.T
        HF = NFF // 2
        with contextlib.ExitStack() as st:
            sb = lambda name, shape, dt_: st.enter_context(nc.sbuf_tensor(self.un(name), shape, dt_))
            wo = sb("wo", [128, 8, D], BF16)
            wg = sb("wg", [128, 8, DFF], BF16)
            wu = sb("wu", [128, 8, DFF], BF16)
            wd = sb("wd", [128, NFF, D], BF16)
            xt = sb("xt", [128, 8, 512], F32)
            hT = sb("hT", [128, HF, 512], BF16)
            xn2 = sb("xn2", [128, 8, 512], BF16)
            sg = [sb("sg%d" % i, [128, 512], BF16) for i in range(2)]
            self.dma("pool", wo[:], w_o_ap.rearrange("(k p) n -> p k n", p=128), [], ["wo"])
            for (wt, src, nm) in ((wg, self.w_fg[l], "wg"), (wu, self.w_fu[l], "wu")):
                v = src.rearrange("(k p) n -> p k n", p=128)
                for hlf in range(2):
                    self.dma("pool", wt[:, :, hlf * 1408:(hlf + 1) * 1408], v[:, :, hlf * 1408:(hlf + 1) * 1408], [], [nm + str(hlf)])
            wdv = self.w_fd[l].rearrange("(f p) n -> p f n", p=128)
            for hlf in range(2):
                self.dma("pool", wd[:, hlf * HF:(hlf + 1) * HF, :], wdv[:, hlf * HF:(hlf + 1) * HF, :], [], ["wd%d" % hlf])
            xv = self.xres.rearrange("(c p) t -> p c t", p=128)
            yv = self.yT.rearrange("(c p) t -> p c t", p=128)
            ov = (self.outT if last else self.xres).rearrange("(c p) t -> p c t", p=128)
            hnames = ["hT%d" % f for f in range(HF)]
            for tt in range(self.NT):
                t0 = tt * 512
                X = xt
                Y = hT[:, 0:8, :]
                self.dma("sp", X[:], xv[:, :, t0:t0 + 512], [], ["xt0"])
                self.dma("sp", Y, yv[:, :, t0:t0 + 512], [], ["yt0"] + hnames)
                for m in range(8):
                    ps = self.ps[m % 2]
                    for k in range(8):
                        self.mm(ps[:], wo[:, k, m * 128:(m + 1) * 128], Y[:, k, :], k == 0, k == 7,
                                ["wo", "yt0"], ["ps%d" % (m % 2)])
                    self.tt("dve", X[:, m, :], ps[:], X[:, m, :], ALU.add, ["ps%d" % (m % 2), "xt0"], ["xt0"])
                gc = [self.C("nf", l * 8 + c) for c in range(8)]
                self.rms_fm([X[:, c, :] for c in range(8)], gc, [xn2[:, c, :] for c in range(8)], 512, D,
                            "c", ["xt0"], [["xn2"]] * 8)
                for hlf in range(2):
                    for fi in range(HF):
                        f = hlf * HF + fi
                        pg, pu = self.ps[2 + (f % 2)], self.ps[4 + (f % 2)]
                        wh = "0" if f * 128 < 1408 else "1"
                        for k in range(8):
                            self.mm(pg[:], wg[:, k, f * 128:(f + 1) * 128], xn2[:, k, :], k == 0, k == 7,
                                    ["wg" + wh, "xn2"], ["ps%d" % (2 + f % 2)])
                        for k in range(8):
                            self.mm(pu[:], wu[:, k, f * 128:(f + 1) * 128], xn2[:, k, :], k == 0, k == 7,
                                    ["wu" + wh, "xn2"], ["ps%d" % (4 + f % 2)])
                        self.act(sg[f % 2][:], pg[:], AF.Silu, ["ps%d" % (2 + f % 2)], ["sg%d" % (f % 2)])
                        self.tt("dve", hT[:, fi, :], sg[f % 2][:], pu[:], ALU.mult, ["sg%d" % (f % 2), "ps%d" % (4 + f % 2)],
                                ["hT%d" % fi, "yt0"])
                    for m in range(8):
                        ps = self.ps[m % 2]
                        for fi in range(HF):
                            f = hlf * HF + fi
                            self.mm(ps[:], wd[:, f, m * 128:(m + 1) * 128], hT[:, fi, :], fi == 0, fi == HF - 1,
                                    ["wd%d" % hlf, "hT%d" % fi], ["ps%d" % (m % 2)])
                        self.tt("dve", X[:, m, :], ps[:], X[:, m, :], ALU.add, ["ps%d" % (m % 2), "xt0"], ["xt0"])
                self.dma("sp", ov[:, :, t0:t0 + 512], X[:], ["xt0"], [])
        self.p.barrier()

    def phase_a(self, l, xnT, src, st):
        nc = self.nc
        xa = [st.enter_context(nc.sbuf_tensor(self.un("xa%d" % i), [128, 8, 512], F32)) for i in range(2)]
        xv = src.rearrange("(c p) t -> p c t", p=128)
        for tt in range(self.NT):
            s = tt % 2
            t0 = tt * 512
            self.dma("sp", xa[s][:], xv[:, :, t0:t0 + 512], [], ["xa%d" % s])
            gc = [self.C("nm", l * 8 + c) for c in range(8)]
            self.rms_fm([xa[s][:, c, :] for c in range(8)], gc, [xnT[:, c, t0:t0 + 512] for c in range(8)], 512, D,
                        "a", ["xa%d" % s], [["xnT%d" % tt]] * 8)

    def phase_moba(self, l, xnT, st):
        nc, T, NT = self.nc, self.T, self.NT
        j = l // 2
        NB = T // 256
        sb = lambda name, shape, dt_: st.enter_context(nc.sbuf_tensor(self.un(name), shape, dt_))
        wq = [sb("wq%d" % i, [128, 8, 384], BF16) for i in range(2)]
        qT = sb("qT", [128, T], BF16)
        kT = sb("kT", [128, T], BF16)
        V = sb("V", [128, T // 128, 128], BF16)
        kmf = sb("kmf", [128, 16], F32)
        kmb = sb("kmb", [128, 16], BF16)
        SELT = sb("SELT", [18, T], BF16)
        gm = sb("gm", [128, 16], F32)
        top8 = sb("top8", [128, 8], F32)
        selb = sb("selb", [128, 16], F32)
        PT = [sb("PT%d" % i, [128, 512], BF16) for i in range(3)]
        rden = [sb("rden%d" % i, [128, 512], F32) for i in range(2)]
        yh = [sb("yh%d" % i, [128, T], BF16) for i in range(2)]
        wv_ = self.w_qkv[j].rearrange("(k p) n -> p k n", p=128)
        yv = self.yT
        for h in range(8):
            hs = h % 2
            W = wq[hs]
            for part in range(3):
                self.dma("pool", W[:, :, part * 128:(part + 1) * 128], wv_[:, :, part * D + h * 128: part * D + (h + 1) * 128],
                         [], ["wq%d_%d" % (hs, part)])
            self.dma("pool", SELT[16:18, :], self.al_in[h], [], ["SELTa"])
            for tt in range(NT):
                t0 = tt * 512
                for qi, (dst, gn) in enumerate(((qT, "qn%d" % j), (kT, "kn%d" % j))):
                    ps = self.ps[qi]
                    for k in range(8):
                        self.mm(ps[:], W[:, k, qi * 128:(qi + 1) * 128], xnT[:, k, t0:t0 + 512], k == 0, k == 7,
                                ["wq%d_%d" % (hs, qi), "xnT%d" % tt], ["ps%d" % qi])
                    self.rms_fm([ps[:]], [self.C(gn)], [dst[:, t0:t0 + 512]], 512, 128, "m", ["ps%d" % qi],
                                [["%sT%d" % ("qk"[qi], tt)]])
                for u in range(4):
                    ti = tt * 4 + u
                    ps = self.ps[2 + (ti % 2)]
                    for k in range(8):
                        self.mm(ps[:, :128], xnT[:, k, ti * 128:(ti + 1) * 128], W[:, k, 256:384], k == 0, k == 7,
                                ["wq%d_2" % hs, "xnT%d" % tt], ["ps%d" % (2 + ti % 2)])
                    self.cp("act", V[:, ti, :], ps[:, :128], ["ps%d" % (2 + ti % 2)], ["V%d" % (ti // 2)])
            self.p.op("dve", lambda e: e.tensor_reduce(out=kmf[:, :NB], in_=kT[:].rearrange("p (n s) -> p n s", s=256),
                                                      axis=AX.X, op=ALU.add),
                      ["kT%d" % tt for tt in range(NT)], ["kmf"])
            self.ts("dve", kmb[:, :NB], kmf[:, :NB], 1.0 / 256.0, None, ALU.mult, None, ["kmf"], ["kmb"])
            if NB < 16:
                self.p.op("dve", lambda e: e.memset(gm[:], -1e30), [], ["gm"])
            for qt in range(T // 128):
                qb = qt // 2
                ps = self.ps[2 + (qt % 2)]
                self.mm(ps[:, :NB], qT[:, qt * 128:(qt + 1) * 128], kmb[:, :NB], True, True,
                        ["qT%d" % (qt // 4), "kmb"], ["ps%d" % (2 + qt % 2)])
                self.tt("dve", gm[:, :NB], ps[:, :NB], self.C("pbias", qb * 16, NB), ALU.add, ["ps%d" % (2 + qt % 2), "CP"], ["gm"])
                self.p.op("dve", lambda e: e.max(out=top8[:], in_=gm[:]), ["gm"], ["top8"])
                self.ts("dve", selb[:], gm[:], top8[:, 2:3], 1.0, ALU.is_ge, ALU.subtract, ["gm", "top8"], ["selb"])
                self.tt("dve", selb[:], selb[:], self.C("pmb", qb * 16, 16), ALU.mult, ["selb", "CP"], ["selb"])
                pt = self.ps[4 + (qt % 2)]
                self.tr(pt[:16, :128], selb[:], self.C("identf", 0, 128), ["selb", "CP"], ["ps%d" % (4 + qt % 2)])
                self.cp("act", SELT[0:16, qt * 128:(qt + 1) * 128], pt[:16, :128], ["ps%d" % (4 + qt % 2)], ["SELT%d" % (qt // 2)])
            tiles = [(qt, kt) for qt in range(T // 512) for kt in range(4 * qt + 4)]
            oE = self.offb["E"][0]

            def issue_scores(i):
                qt, kt = tiles[i]
                t0 = qt * 512
                bank = i % 2
                pss = self.ps[bank]
                diag = kt >= 4 * qt
                self.mm(pss[:, :], kT[:, kt * 128:(kt + 1) * 128], qT[:, t0:t0 + 512], True, False,
                        ["kT%d" % (kt // 4), "qT%d" % qt], ["ps%d" % bank])
                self.mm(pss[:, :], self.CB[0:18, oE + (kt // 2) * 128:oE + (kt // 2 + 1) * 128], SELT[0:18, t0:t0 + 512],
                        False, not diag, ["CB", "SELT%d" % (2 * qt), "SELT%d" % (2 * qt + 1), "SELTa"], ["ps%d" % bank])
                if diag:
                    self.mm(pss[:, :], self.CB_("ident"), self.CB_("cm", (kt - 4 * qt) * 512, 512), False, True,
                            ["CB"], ["ps%d" % bank])

            def issue_rest(i):
                qt, kt = tiles[i]
                t0 = qt * 512
                bank = i % 2
                pss = self.ps[bank]
                nk = 4 * qt + 4
                ob, db = 2 + 2 * (qt % 2), 3 + 2 * (qt % 2)
                pso, psd = self.ps[ob], self.ps[db]
                P = PT[i % 3]
                r = 4 * qt - kt
                self.act(P[:], pss[:, :], AF.Exp, ["ps%d" % bank, "CP"], ["PT%d" % (i % 3)],
                         bias=self.C("bpt", h * 34 + r + 3))
                self.mm(pso[:, :], V[:, kt, :], P[:], kt == 0, kt == nk - 1, ["V%d" % (kt // 2), "PT%d" % (i % 3)], ["ps%d" % ob])
                self.mm(psd[:, :], self.CB_("ones"), P[:], kt == 0, kt == nk - 1, ["CB", "PT%d" % (i % 3)], ["ps%d" % db])
                if kt == nk - 1:
                    rd = rden[qt % 2]
                    self.p.op("dve", lambda e: e.reciprocal(out=rd[:], in_=psd[:, :]), ["ps%d" % db], ["rden%d" % (qt % 2)])
                    self.tt("dve", yh[hs][:, t0:t0 + 512], pso[:, :], rd[:], ALU.mult, ["ps%d" % ob, "rden%d" % (qt % 2)], ["yh%d" % hs])

            issue_scores(0)
            for i in range(len(tiles)):
                if i + 1 < len(tiles):
                    issue_scores(i + 1)
                issue_rest(i)
            self.dma("sp", yv[h * 128:(h + 1) * 128, :], yh[hs][:], ["yh%d" % hs], ["yT"])

    def phase_hybrid(self, l, xnT, st):
        nc, T, NT = self.nc, self.T, self.NT
        j = l // 2
        NCH = T // 128
        sb = lambda name, shape, dt_: st.enter_context(nc.sbuf_tensor(self.un(name), shape, dt_))
        wq = sb("hwq", [128, 8, 128], BF16)
        wk = sb("hwk", [128, 8, 128], BF16)
        wv = sb("hwv", [128, 8, 256], BF16)
        wgt = sb("hwg", [128, 8, 256], BF16)
        wlr = sb("hwlr", [128, 8, 16], BF16)
        wif = sb("hwif", [128, 8, 8], BF16)
        wg2 = sb("hwg2", [16, 256], BF16)
        qT = sb("hqT", [128, T], BF16)
        kT = sb("hkT", [128, T], BF16)
        QR = [sb("hQR%d" % i, [128, 515], F32) for i in range(2)]
        ACC0 = sb("hACC0", [128, 512], F32)
        ACC = [ACC0, ACC0]
        V = sb("hV", [128, NCH, 256], BF16)
        GA = sb("hGA", [128, 2, 512], BF16)
        lrT = sb("hlrT", [16, T], BF16)
        GT8 = sb("hGT8", [8, T], BF16)
        LF8 = lrT[0:8, :]
        LFt = sb("hLFt", [8, 512], F32)
        Bg = sb("hBg", [128, T + 1], F32)
        NBt = sb("hNB", [128, T], F32)
        BC = sb("hBC", [128, NCH + 1], F32)
        NBC = sb("hNBC", [128, NCH + 1], F32)
        onesf = sb("honesf", [128, 512], BF16)
        E1 = [sb("hE1_%d" % i, [128, 128], F32) for i in range(2)]
        E2 = sb("hE2", [128, 128], F32)
        E3 = sb("hE3", [128, 128], F32)
        qc = [sb("hqc%d" % i, [128, 128], BF16) for i in range(2)]
        kc = [sb("hkc%d" % i, [128, 128], BF16) for i in range(2)]
        kh = sb("hkh", [128, 128], BF16)
        khT = [sb("hkhT%d" % i, [128, 128], BF16) for i in range(2)]
        AT = [sb("hAT%d" % i, [128, 128], BF16) for i in range(4)]
        S32 = sb("hS32", [128, 128], F32)
        Sb = sb("hSb", [128, 128], BF16)
        N32 = sb("hN32", [128, 128], F32)
        Nb = sb("hNb", [128, 128], BF16)
        OT = sb("hOT", [128, 2, 512], F32)
        DD = sb("hDD", [128, 2, 128], F32)
        yo = [sb("hyo%d" % i, [128, 2, 512], BF16) for i in range(2)]
        psb = self.psb
        win = self.w_in[j].rearrange("(k p) n -> p k n", p=128)
        yv = self.yT
        osel = self.offb["sel"][0]

        def load_w(dst, c0, n, nm):
            self.dma("pool", dst[:, :, :n], win[:, :, c0:c0 + n], [], [nm])

        self.p.op("dve", lambda e: e.memset(onesf[:], 1.0), [], ["onesf"])
        self.p.op("dve", lambda e: e.memset(Bg[:, 0:1], 0.0), [], ["Bg0"])
        load_w(wlr, 1536, 16, "hwlr")
        load_w(wif, 3088, 8, "hwif")
        self.dma("pool", wg2[:], self.w_g2[j], [], ["hwg2"])
        obif = self.off["bif%d" % j][0]
        for tt in range(NT):
            t0 = tt * 512
            ps = self.ps[0]
            for k in range(8):
                self.mm(ps[:16, :], wlr[:, k, :], xnT[:, k, t0:t0 + 512], k == 0, k == 7, ["hwlr", "xnT%d" % tt], ["ps0"])
            self.cp("act", lrT[:, t0:t0 + 512], ps[:16, :], ["ps0"], ["hlrT%d" % tt])

        def gate_rows():
            for tt in range(NT):
                t0 = tt * 512
                ps = self.ps[1]
                for k in range(8):
                    self.mm(ps[:8, :], wif[:, k, :], xnT[:, k, t0:t0 + 512], k == 0, k == 7, ["hwif", "xnT%d" % tt], ["ps1"])
                self.act(GT8[:, t0:t0 + 512], ps[:8, :], AF.Identity, ["ps1", "CP"], ["hGT8"], bias=self.CP[0:8, obif:obif + 1])
                self.act(LFt[:], ps[:8, :], AF.Identity, ["ps1", "CP"], ["hLFt"], bias=self.CP[0:8, obif:obif + 1])
                self.act(LFt[:], LFt[:], AF.Exp, ["hLFt"], ["hLFt"], scale=-1.0)
                self.act(LFt[:], LFt[:], AF.Ln, ["hLFt", "CP"], ["hLFt"], bias=self.CP[0:8, self.off["scr"][0] + 1:self.off["scr"][0] + 2])
                self.ts("dve", LF8[:, t0:t0 + 512], LFt[:], -1.0, None, ALU.mult, None, ["hLFt"], ["hLF8"])

        for pair in range(4):
            ml = pair >= 2
            pp = pair % 2
            if pair == 2:
                gate_rows()
            if not ml:
                cq, ck, cv, cg = 128 * pp, 256 + 128 * pp, 512 + 256 * pp, 1024 + 256 * pp
            else:
                cq, ck, cv, cg = 1552 + 128 * pp, 1808 + 128 * pp, 2064 + 256 * pp, 2576 + 256 * pp
            load_w(wq, cq, 128, "hwq")
            load_w(wk, ck, 128, "hwk")
            load_w(wv, cv, 256, "hwv")
            load_w(wgt, cg, 256, "hwg")
            gnm = ("mn%d" if ml else "gn%d") % j
            for qi, (wt, dst, nm) in enumerate(((wq, qT, "hwq"), (wk, kT, "hwk"))):
                dn = "h%sT" % "qk"[qi]
                for tt in range(NT):
                    t0 = tt * 512
                    s = tt % 2
                    ps = self.ps[tt % 2]
                    for k in range(8):
                        self.mm(ps[:], wt[:, k, :], xnT[:, k, t0:t0 + 512], k == 0, k == 7, [nm, "xnT%d" % tt], ["ps%d" % (tt % 2)])
                    if not ml:
                        self.cp("act", dst[:, t0:t0 + 512], ps[:], ["ps%d" % (tt % 2)], [dn])
                    else:
                        self.cp("act", QR[s][:, 3:515], ps[:], ["ps%d" % (tt % 2)], ["hQR%d" % s])
                        if tt == 0:
                            self.p.op("dve", lambda e, s=s: e.memset(QR[s][:, 0:3], 0.0), [], ["hQRh%d" % s])
                        else:
                            self.cp("dve", QR[s][:, 0:3], QR[1 - s][:, 512:515], ["hQR%d" % (1 - s)], ["hQRh%d" % s])
                        o = self.off["cw%d" % j][0] + pp * 8 + qi * 4
                        rd = ["hQR%d" % s, "hQRh%d" % s, "CP"]
                        self.ts("dve", ACC[s][:], QR[s][:, 0:512], self.CP[:, o:o + 1], None, ALU.mult, None, rd, ["hACC0"])
                        for tap in range(1, 4):
                            self.stt(ACC[s][:], QR[s][:, tap:tap + 512], self.CP[:, o + tap:o + tap + 1], ACC[s][:], ALU.mult, ALU.add,
                                     rd + ["hACC0"], ["hACC0"])
                        self.act(dst[:, t0:t0 + 512], ACC[s][:], AF.Silu, ["hACC0"], [dn])
            for tt in range(NT):
                t0 = tt * 512
                for u in range(4):
                    ti = tt * 4 + u
                    ps = self.ps[4 + (ti % 2)]
                    for k in range(8):
                        self.mm(ps[:, :256], xnT[:, k, ti * 128:(ti + 1) * 128], wv[:, k, :], k == 0, k == 7,
                                ["hwv", "xnT%d" % tt], ["ps%d" % (4 + ti % 2)])
                    self.cp("act", V[:, ti, :], ps[:, :256], ["ps%d" % (4 + ti % 2)], ["hV"])
            for tt in range(NT):
                t0 = tt * 512
                ps = self.ps[2]
                if not ml:
                    self.mm(ps[:], wg2[:, pp * 128:(pp + 1) * 128], lrT[:, t0:t0 + 512], True, True, ["hwg2", "hlrT%d" % tt], ["ps2"])
                    self.act(NBt[:, t0:t0 + 512], ps[:], AF.Exp, ["ps2", "CP"], ["hNB"], bias=self.C("gb%d" % j, pp), scale=-1.0)
                    self.act(NBt[:, t0:t0 + 512], NBt[:, t0:t0 + 512], AF.Ln, ["hNB", "CP"], ["hNB"], bias=self.C("scr", 1))
                    self.ts("dve", NBt[:, t0:t0 + 512], NBt[:, t0:t0 + 512], -1.0 / 16.0, None, ALU.mult, None, ["hNB"], ["hNB"])
                else:
                    o = osel + (pp * 2 + 0) * 128
                    self.mm(ps[:], self.CB[0:8, o:o + 128], LF8[:, t0:t0 + 512], True, True, ["CB", "hLF8"], ["ps2"])
                    self.cp("act", NBt[:, t0:t0 + 512], ps[:], ["ps2"], ["hNB"])
                ini = Bg[:, t0:t0 + 1]
                self.p.op("dve", lambda e, t0=t0, ini=ini: e.tensor_tensor_scan(out=Bg[:, 1 + t0:1 + t0 + 512], data0=onesf[:],
                                                                                   data1=NBt[:, t0:t0 + 512], initial=ini,
                                                                                   op0=ALU.mult, op1=ALU.add),
                          ["hNB", "onesf", "hBg", "Bg0"], ["hBg"])
            self.cp("dve", BC[:, 0:NCH], Bg[:, 0:T].rearrange("p (n s) -> p n s", s=128)[:, :, 0], ["hBg", "Bg0"], ["hBC"])
            self.cp("dve", BC[:, NCH:NCH + 1], Bg[:, T:T + 1], ["hBg", "Bg0"], ["hBC"])
            self.ts("dve", NBC[:, :], BC[:, :], -1.0, None, ALU.mult, None, ["hBC"], ["hNBC"])
            for tt in range(NT):
                t0 = tt * 512
                if not ml:
                    self.ts("dve", NBt[:, t0:t0 + 512], Bg[:, 1 + t0:1 + t0 + 512], -1.0, None, ALU.mult, None, ["hBg"], ["hNB"])
                else:
                    ps = self.ps[2]
                    o = osel + (pp * 2 + 1) * 128
                    self.mm(ps[:], self.CB[0:8, o:o + 128], GT8[:, t0:t0 + 512], True, True, ["CB", "hGT8"], ["ps2"])
                    self.tt("dve", NBt[:, t0:t0 + 512], ps[:], Bg[:, 1 + t0:1 + t0 + 512], ALU.subtract, ["ps2", "hBg"], ["hNB"])
            self.p.barrier()
            self.p.op("dve", lambda e: e.memset(S32[:], 0.0), [], ["hS32"])
            self.p.op("dve", lambda e: e.memset(Sb[:], 0.0), [], ["hSb"])
            if ml:
                self.p.op("dve", lambda e: e.memset(N32[:], 0.0), [], ["hN32"])
                self.p.op("dve", lambda e: e.memset(Nb[:], 0.0), [], ["hNb"])
            def stage1(ci):
                c0 = ci * 128
                s2 = ci % 2
                sl = slice(c0, c0 + 128)
                e1 = E1[s2]
                self.act(e1[:], Bg[:, 1 + c0:1 + c0 + 128], AF.Exp, ["hBg", "hNBC"], ["hE1_%d" % s2], bias=NBC[:, ci:ci + 1])
                self.act(E2[:], NBt[:, sl], AF.Exp, ["hNB", "hBC"], ["hE2"], bias=BC[:, ci:ci + 1])
                self.act(E3[:], NBt[:, sl], AF.Exp, ["hNB", "hBC"], ["hE3"], bias=BC[:, ci + 1:ci + 2])
                self.stt(qc[s2][:], qT[:, sl], 0.125, e1[:], ALU.mult, ALU.mult, ["hqT", "hE1_%d" % s2], ["hqc%d" % s2])
                self.tt("pool", kc[s2][:], kT[:, sl], E2[:], ALU.mult, ["hkT", "hE2"], ["hkc%d" % s2])
                self.tt("pool", kh[:], kT[:, sl], E3[:], ALU.mult, ["hkT", "hE3"], ["hkh"])
                self.tr(psb[:, :128], kh[:], self.CB_("ident"), ["hkh", "CB"], ["psb"])
                self.cp("act", khT[s2][:], psb[:, :128], ["psb"], ["hkhT%d" % s2])
                for hh in range(2):
                    hp = slice(64 * hh, 64 * hh + 64)
                    self.mm(self.ps[hh][:, 0:128], kc[s2][hp, :], qc[s2][hp, :], True, True, ["hkc%d" % s2, "hqc%d" % s2], ["ps%d" % hh])
                for hh in range(2):
                    self.tt("dve", AT[s2 * 2 + hh][:], self.ps[hh][:, 0:128], self.CB_("tri"), ALU.mult, ["ps%d" % hh, "CB"], ["hAT%d" % (s2 * 2 + hh)])

            stage1(0)
            for ci in range(NCH):
                c0 = ci * 128
                s2 = ci % 2
                u = ci % 4
                sl = slice(c0, c0 + 128)
                e1 = E1[s2]
                if ci + 1 < NCH:
                    stage1(ci + 1)
                psU = self.ps[5]
                for hh in range(2):
                    hp = slice(64 * hh, 64 * hh + 64)
                    psO, psD = self.ps[2 + hh], self.ps[4]
                    nO, nD = "ps%d" % (2 + hh), "ps4"
                    at = AT[s2 * 2 + hh]
                    an = "hAT%d" % (s2 * 2 + hh)
                    self.mm(psO[:, 0:128], V[:, ci, hh * 128:(hh + 1) * 128], at[:], True, False, ["hV", an], [nO])
                    self.mm(psO[:, 0:128], Sb[hp, :], qc[s2][hp, :], False, True, ["hSb", "hqc%d" % s2], [nO])
                    if ml:
                        self.mm(psD[:, hh * 128:(hh + 1) * 128], self.CB_("ones"), at[:], True, False, ["CB", an], [nD])
                        self.mm(psD[:, hh * 128:(hh + 1) * 128], Nb[hp, :], qc[s2][hp, :], False, True, ["hNb", "hqc%d" % s2], [nD])
                    self.mm(psU[hp, 0:128], khT[s2][:, hp], V[:, ci, hh * 128:(hh + 1) * 128], True, True, ["hkhT%d" % s2, "hV"], ["ps5"])
                    if ml:
                        self.mm(psU[hp, 128:256], khT[s2][:, hp], self.CB_("ones"), True, True, ["hkhT%d" % s2, "CB"], ["ps5"])
                    if not ml:
                        self.cp("act", OT[:, hh, u * 128:(u + 1) * 128], psO[:, 0:128], [nO], ["hOT"])
                    else:
                        self.act(DD[:, hh, :], psD[:, hh * 128:(hh + 1) * 128], AF.Abs, [nD], ["hDD%d" % hh])
                        self.ts("dve", DD[:, hh, :], DD[:, hh, :], 1.0, None, ALU.max, None, ["hDD%d" % hh], ["hDD%d" % hh])
                        self.p.op("dve", lambda e, hh=hh: e.reciprocal(out=DD[:, hh, :], in_=DD[:, hh, :]), ["hDD%d" % hh], ["hDD%d" % hh])
                        self.tt("dve", OT[:, hh, u * 128:(u + 1) * 128], psO[:, 0:128], DD[:, hh, :], ALU.mult, [nO, "hDD%d" % hh], ["hOT"])
                self.stt(S32[:], S32[:], e1[:, 127:128], psU[:, 0:128], ALU.mult, ALU.add, ["hS32", "hE1_%d" % s2, "ps5"], ["hS32"])
                self.cp("act", Sb[:], S32[:], ["hS32"], ["hSb"])
                if ml:
                    self.stt(N32[:], N32[:], e1[:, 127:128], psU[:, 128:256], ALU.mult, ALU.add, ["hN32", "hE1_%d" % s2, "ps5"], ["hN32"])
                    self.cp("act", Nb[:], N32[:], ["hN32"], ["hNb"])
                if u == 3:
                    tt = ci // 4
                    t0 = tt * 512
                    Y = yo[tt % 2]
                    for hh in range(2):
                        head = 2 * pp + hh
                        ps = self.ps[hh]
                        for k in range(8):
                            self.mm(ps[:], wgt[:, k, hh * 128:(hh + 1) * 128], xnT[:, k, t0:t0 + 512], k == 0, k == 7,
                                    ["hwg", "xnT%d" % tt], ["ps%d" % hh])
                        self.act(GA[:, hh, :], ps[:], AF.Sigmoid if ml else AF.Silu, ["ps%d" % hh], ["hGA%d" % hh])
                        if ml:
                            self.tt("pool", OT[:, hh, :], OT[:, hh, :], GA[:, hh, :], ALU.mult, ["hOT", "hGA%d" % hh], ["hOT"])
                            self.rms_fm([OT[:, hh, :]], [self.C(gnm, head)], [Y[:, hh, :]], 512, 128, "h", ["hOT"], [["hyo%d" % (tt % 2)]])
                        else:
                            self.rms_fm([OT[:, hh, :]], [self.C(gnm, head)], [OT[:, hh, :]], 512, 128, "h", ["hOT"], [["hOT"]])
                            self.tt("pool", Y[:, hh, :], OT[:, hh, :], GA[:, hh, :], ALU.mult, ["hOT", "hGA%d" % hh], ["hyo%d" % (tt % 2)])
                    r0 = (512 if ml else 0) + pp * 256
                    self.dma("sp", yv[r0:r0 + 256, t0:t0 + 512].rearrange("(h p) t -> p h t", p=128), Y[:], ["hyo%d" % (tt % 2)], ["yT"])
            self.p.barrier()


    def build(self):
        nc = self.nc
        with contextlib.ExitStack() as st:
            self.setup(st)
            first = True
            for li, l in enumerate(self.layers):
                src = self.xT_in if first else self.xres
                with contextlib.ExitStack() as st2:
                    xnT = st2.enter_context(nc.sbuf_tensor(self.un("xnT"), [128, 8, self.T], BF16))
                    with contextlib.ExitStack() as st3:
                        self.phase_a(l, xnT, src, st3)
                    self.p.barrier()
                    with contextlib.ExitStack() as st3:
                        if l % 2 == 0:
                            self.phase_hybrid(l, xnT, st3)
                        else:
                            self.phase_moba(l, xnT, st3)
                    self.p.barrier()
                if first and self.dbg != "y":
                    self.dma("sp", self.xres, self.xT_in, [], ["xres_all"])
                    self.p.barrier()
                first = False
                w_o = (self.w_ho if l % 2 == 0 else self.w_mo)[l // 2]
                if self.dbg == "y":
                    self.dma("pool", self.outT, self.yT, [], [])
                    break
                self.phase_c(l, w_o, last=(li == len(self.layers) - 1))
            self.p.emit()
        return nc


_CACHE = {}


def _get_nc(T):
    if T not in _CACHE:
        _CACHE[T] = K(T).build()
    return _CACHE[T]


def make_in_maps(inputs, T, ncores=8):
    cp, cb, al = _pack_consts(T, inputs)
    x = np.asarray(inputs["x"], np.float32)
    nb = x.shape[0]
    shared = {k: np.ascontiguousarray(np.asarray(inputs[k], np.float32)) for k in
              ("hyb_w_in", "gla_w_gate2", "hyb_w_out", "moba_w_qkv", "moba_w_o", "ffn_w_gate", "ffn_w_up", "ffn_w_down")}
    maps = []
    for c in range(ncores):
        b = c % nb
        m = dict(shared)
        m["xT"] = np.ascontiguousarray(x[b].T)
        m["cpack"] = cp
        m["cbf"] = cb
        m["alrow"] = al
        maps.append(m)
    return maps


def kernel(**inputs):
    x = np.asarray(inputs["x"])
    B, T, _ = x.shape
    nc = _get_nc(T)
    maps = make_in_maps(inputs, T)
    res = run_bass_kernel_spmd(nc, maps, core_ids=list(range(8)))
    out = np.stack([np.ascontiguousarray(res.results[b]["outT"].T) for b in range(B)], axis=0)
    return out.astype(np.float32)
```
